# Optimizing a Trainium2 kernel written in Bass

```python
import jax, jax.numpy as jnp
from jax import lax
import numpy as np

D_MODEL = 2048
BATCH = 2
SEQ = 4096
DEPTH = 1

CHUNK = 64
Q_BLOCK = 128
PLE_DIM = 256
EPS = 1e-6
MASK_VALUE = -1e30

MLA_HEADS = 8
QK_NOPE = 128
QK_ROPE = 64
QK_HEAD = QK_NOPE + QK_ROPE
V_HEAD = 128
Q_RANK = 512
KV_RANK = 256
ROPE_BASE = 10000.0
D_ATTN = MLA_HEADS * V_HEAD

D_CONV = D_MODEL // 2
CONV_WIDTH = 3

D_MIX = D_ATTN + D_CONV
D_IN = Q_RANK + KV_RANK + QK_ROPE + 3 * D_CONV

N_GROUPS = 4
EXPERTS_PER_GROUP = 8
N_EXPERTS = N_GROUPS * EXPERTS_PER_GROUP
TOP_K = 2
D_EXPERT = 512

kernel_name = "hybrid_mla_shortconv_hmoe_ple"


def rms_norm(x, g):
    xf = x.astype(jnp.float32)
    y = xf * lax.rsqrt(jnp.mean(xf * xf, axis=-1, keepdims=True) + EPS)
    return (y * g.astype(jnp.float32)).astype(x.dtype)


def rope_tables(positions, dtype):
    freq = ROPE_BASE ** (-jnp.arange(0, QK_ROPE, 2, dtype=jnp.float32) / QK_ROPE)
    ang = positions.astype(jnp.float32)[..., None] * freq
    return jnp.cos(ang).astype(dtype), jnp.sin(ang).astype(dtype)


def apply_rope(x, cos, sin):
    half = x.shape[-1] // 2
    x1, x2 = x[..., :half], x[..., half:]
    return jnp.concatenate([x1 * cos - x2 * sin, x2 * cos + x1 * sin], axis=-1)


def mla_mixer(q_lat, kv_lat, k_rope, positions, g_q_lat, g_kv_lat, w_uq, w_ukv, g_q_head, g_k_head):
    B, S, _ = q_lat.shape
    H = MLA_HEADS
    q = jnp.einsum('bsr,rhd->bshd', rms_norm(q_lat, g_q_lat), w_uq)
    kv = jnp.einsum('bsr,rhd->bshd', rms_norm(kv_lat, g_kv_lat), w_ukv)
    k_nope, v = kv[..., :QK_NOPE], kv[..., QK_NOPE:]
    k = jnp.concatenate([k_nope, jnp.broadcast_to(k_rope[:, :, None, :], (B, S, H, QK_ROPE))], axis=-1)
    q = rms_norm(q, g_q_head)
    k = rms_norm(k, g_k_head)
    cos, sin = rope_tables(positions, q.dtype)
    cos, sin = cos[:, :, None, :], sin[:, :, None, :]
    q = jnp.concatenate([q[..., :QK_NOPE], apply_rope(q[..., QK_NOPE:], cos, sin)], axis=-1)
    k = jnp.concatenate([k[..., :QK_NOPE], apply_rope(k[..., QK_NOPE:], cos, sin)], axis=-1)

    nb = S // Q_BLOCK
    qb = q.reshape(B, nb, Q_BLOCK, H, QK_HEAD).transpose(1, 0, 3, 2, 4)
    kT = k.transpose(0, 2, 1, 3)
    vT = v.transpose(0, 2, 1, 3)
    key_chunk = jnp.arange(S) // CHUNK
    scale = QK_HEAD ** -0.5

    def attend_block(args):
        qi, i = args
        s = jnp.einsum('bhqd,bhkd->bhqk', qi, kT).astype(jnp.float32) * scale
        q_chunk = (i * Q_BLOCK + jnp.arange(Q_BLOCK)) // CHUNK
        mask = key_chunk[None, :] <= q_chunk[:, None]
        s = jnp.where(mask, s, MASK_VALUE)
        pr = jax.nn.softmax(s, axis=-1).astype(vT.dtype)
        return jnp.einsum('bhqk,bhkd->bhqd', pr, vT)

    out = lax.map(attend_block, (qb, jnp.arange(nb)))
    return out.transpose(1, 0, 3, 2, 4).reshape(B, S, D_ATTN)


def short_conv_mixer(b_gate, c_gate, h, w_conv):
    S = h.shape[1]
    u = c_gate * h
    u_pad = jnp.pad(u, ((0, 0), (CONV_WIDTH - 1, 0), (0, 0)))
    conv = sum(u_pad[:, j:j + S] * w_conv[j] for j in range(CONV_WIDTH))
    return b_gate * conv


def hier_moe(x, w_group, b_group, w_router, b_router, w1, w3, w2):
    B, S, D = x.shape
    t = x.reshape(-1, D)
    T = t.shape[0]
    g_logits = (t @ w_group + b_group).astype(jnp.float32)
    g_prob = jax.nn.softmax(g_logits, axis=-1)
    g_sel = jnp.argmax(g_logits, axis=-1)
    p_g = jnp.take_along_axis(g_prob, g_sel[:, None], axis=-1)
    e_logits = (t @ w_router + b_router).astype(jnp.float32).reshape(T, N_GROUPS, EXPERTS_PER_GROUP)
    idx = jnp.broadcast_to(g_sel[:, None, None], (T, 1, EXPERTS_PER_GROUP))
    e_in_group = jnp.take_along_axis(e_logits, idx, axis=1)[:, 0]
    top_vals, top_idx = lax.top_k(e_in_group, TOP_K)
    w_top = jax.nn.softmax(top_vals, axis=-1) * p_g
    expert_id = g_sel[:, None] * EXPERTS_PER_GROUP + top_idx
    combine = jnp.sum(jax.nn.one_hot(expert_id, N_EXPERTS, dtype=jnp.float32) * w_top[..., None], axis=1)

    def expert_step(acc, params):
        w1e, w3e, w2e, ce = params
        hid = jax.nn.silu(t @ w1e) * (t @ w3e)
        return acc + ce[:, None].astype(t.dtype) * (hid @ w2e), None

    y, _ = lax.scan(expert_step, jnp.zeros_like(t), (w1, w3, w2, combine.T))
    return y.reshape(B, S, D)


def setup_inputs(seed: int = 0) -> dict:
    key = jax.random.key(seed)
    ks = jax.random.split(key, 32)
    f32 = jnp.float32

    def nrm(k, shape, fan_in):
        return jax.random.normal(k, shape, f32) * (fan_in ** -0.5)

    def gain(k, shape):
        return 1.0 + 0.01 * jax.random.normal(k, shape, f32)

    L = DEPTH
    x = jax.random.normal(ks[0], (BATCH, SEQ, D_MODEL), f32)
    p = jax.random.normal(ks[1], (DEPTH, BATCH, SEQ, PLE_DIM), f32)
    offsets = jax.random.randint(ks[2], (BATCH, 1), 0, 65536, dtype=jnp.int32)
    positions = offsets + jnp.arange(SEQ, dtype=jnp.int32)[None, :]
    return {
        "x": x,
        "p": p,
        "positions": positions,
        "norm_mix": gain(ks[3], (L, D_MODEL)),
        "w_in": nrm(ks[4], (L, D_MODEL, D_IN), D_MODEL),
        "g_q_lat": gain(ks[5], (L, Q_RANK)),
        "g_kv_lat": gain(ks[6], (L, KV_RANK)),
        "w_uq": nrm(ks[7], (L, Q_RANK, MLA_HEADS, QK_HEAD), Q_RANK),
        "w_ukv": nrm(ks[8], (L, KV_RANK, MLA_HEADS, QK_NOPE + V_HEAD), KV_RANK),
        "g_q_head": gain(ks[9], (L, QK_HEAD)),
        "g_k_head": gain(ks[10], (L, QK_HEAD)),
        "w_conv": nrm(ks[11], (L, CONV_WIDTH, D_CONV), CONV_WIDTH),
        "g_out_attn": gain(ks[12], (L, D_ATTN)),
        "g_out_conv": gain(ks[13], (L, D_CONV)),
        "w_out": nrm(ks[14], (L, D_MIX, D_MODEL), D_MIX),
        "norm_moe": gain(ks[15], (L, D_MODEL)),
        "w_group": nrm(ks[16], (L, D_MODEL, N_GROUPS), D_MODEL),
        "b_group": 0.01 * jax.random.normal(ks[17], (L, N_GROUPS), f32),
        "w_router": nrm(ks[18], (L, D_MODEL, N_EXPERTS), D_MODEL),
        "b_router": 0.01 * jax.random.normal(ks[19], (L, N_EXPERTS), f32),
        "w1": nrm(ks[20], (L, N_EXPERTS, D_MODEL, D_EXPERT), D_MODEL),
        "w3": nrm(ks[21], (L, N_EXPERTS, D_MODEL, D_EXPERT), D_MODEL),
        "w2": nrm(ks[22], (L, N_EXPERTS, D_EXPERT, D_MODEL), D_EXPERT),
        "norm_ple": gain(ks[23], (L, D_MODEL)),
        "w_ple": nrm(ks[24], (L, PLE_DIM, D_MODEL), PLE_DIM),
        "w_ple_gate": nrm(ks[25], (L, D_MODEL, D_MODEL), D_MODEL),
        "b_ple_gate": 0.01 * jax.random.normal(ks[26], (L, D_MODEL), f32),
    }


def reference(x, p, positions, norm_mix, w_in, g_q_lat, g_kv_lat, w_uq, w_ukv, g_q_head, g_k_head,
              w_conv, g_out_attn, g_out_conv, w_out, norm_moe, w_group, b_group, w_router, b_router,
              w1, w3, w2, norm_ple, w_ple, w_ple_gate, b_ple_gate):
    h = x
    splits = np.cumsum([Q_RANK, KV_RANK, QK_ROPE, D_CONV, D_CONV]).tolist()
    for i in range(DEPTH):
        xn = rms_norm(h, norm_mix[i])
        proj = xn @ w_in[i]
        q_lat, kv_lat, k_rope, b_gate, c_gate, h_conv = jnp.split(proj, splits, axis=-1)
        y_attn = mla_mixer(q_lat, kv_lat, k_rope, positions, g_q_lat[i], g_kv_lat[i],
                           w_uq[i], w_ukv[i], g_q_head[i], g_k_head[i])
        y_conv = short_conv_mixer(b_gate, c_gate, h_conv, w_conv[i])
        y_mix = jnp.concatenate([rms_norm(y_attn, g_out_attn[i]), rms_norm(y_conv, g_out_conv[i])], axis=-1)
        h = h + y_mix @ w_out[i]
        h = h + hier_moe(rms_norm(h, norm_moe[i]), w_group[i], b_group[i], w_router[i], b_router[i],
                         w1[i], w3[i], w2[i])
        gate = jax.nn.sigmoid(rms_norm(h, norm_ple[i]) @ w_ple_gate[i] + b_ple_gate[i])
        h = h + gate * (p[i] @ w_ple[i])
    return h
```

```python
import math
from contextlib import ExitStack

import numpy as np
import concourse.bass as bass
import concourse.mybir as mybir
from concourse.bass_utils import run_bass_kernel_spmd

F32 = mybir.dt.float32
BF16 = mybir.dt.bfloat16
I32 = mybir.dt.int32
AF = mybir.ActivationFunctionType
ALU = mybir.AluOpType
AX = mybir.AxisListType

D = 2048
S = 4096
NB_OWN = 8
TOK = 1024
HALO = 16
H = 8
EPS = 1e-6
NE = 32
DE = 512
TWO_PI = 2.0 * math.pi
SCALE = 192.0 ** -0.5
BIG = 1.0e30

SB_LO = 16512
N_WARM = 16
N_FILL = 2
import os
SB_HI = 229344

R_NMIX, R_GOA, R_NMOE, R_NPLE, R_BPLE, R_BR = 0, 2048, 3072, 5120, 7168, 9216
NROW = 9216 + 36
C_GKV, C_GQL, C_GQN, C_GKN, C_GQR, C_GQRS, C_GKR, C_GKRS, C_FREQ, C_SGN, C_WCONV, C_GOC, C_PHASE = 0, 2, 6, 7, 8, 9, 10, 11, 12, 13, 14, 38, 46
NCOL = 48


class Prog:
    ENG = ("pe", "act", "dve", "pool", "sp")

    def __init__(self, nc, es, nds=40):
        self.nc = nc
        self.e = {"pe": nc.tensor, "act": nc.scalar, "dve": nc.vector, "pool": nc.gpsimd, "sp": nc.sync}
        self.sem = {k: es.enter_context(nc.semaphore("s_" + k)) for k in self.ENG}
        self.cnt = {k: 0 for k in self.ENG}
        self.pending = {k: False for k in self.ENG}
        self.seen = {k: {} for k in self.ENG}
        self.lastw = {}
        self.readers = {}
        self.dsem = [es.enter_context(nc.semaphore(f"d{i}")) for i in range(nds)]
        self.dval = [0] * nds
        self.dnext_q = {k: 0 for k in self.ENG}
        self.nsp = 16
        self.off = SB_LO
        self.nalloc = 0
        self.ninst = 0
        self.bmarks = []
        self.hmarks = []

    def alloc(self, name, shape, dtype):
        nbytes = int(np.prod(shape[1:])) * (4 if dtype in (F32, I32) else 2)
        nbytes = (nbytes + 63) // 64 * 64
        assert self.off + nbytes <= SB_HI, f"SBUF overflow at {name}: {self.off + nbytes - SB_HI}"
        self.nalloc += 1
        t = self.nc.alloc_sbuf_tensor_at(f"{name}_{self.nalloc}", list(shape), dtype, offset=self.off)
        self.off += nbytes
        return t.ap()

    def mark(self):
        return self.off

    def release(self, m):
        self.off = m

    def _wait(self, eng, tok):
        if tok[0] == "e":
            _, e2, idx = tok
            if e2 == eng and eng == "pe":
                return
            key = ("e", e2)
            if self.seen[eng].get(key, 0) >= idx:
                return
            self.seen[eng][key] = idx
            self.e[eng].wait_ge(self.sem[e2], idx)
        else:
            _, s, val = tok
            key = ("d", s)
            if self.seen[eng].get(key, 0) >= val:
                return
            self.seen[eng][key] = val
            self.e[eng].wait_ge(self.dsem[s], val)

    def _deps(self, r, w, eng=None):
        toks = []
        for k in r:
            t = self.lastw.get(k)
            if t is not None:
                toks.append(t)
            if isinstance(k, tuple) and k[0] == "ps" and eng in ("act", "dve"):
                for rk, rt in self.readers.get(k, {}).items():
                    if rk[0] == "e" and rk[1] in ("act", "dve") and rk[1] != eng:
                        toks.append(rt)
        for k in w:
            t = self.lastw.get(k)
            if t is not None:
                toks.append(t)
            toks.extend(self.readers.get(k, {}).values())
        return toks

    def _record(self, tok, r, w):
        for k in w:
            self.lastw[k] = tok
            self.readers[k] = {}
        for k in r:
            d = self.readers.setdefault(k, {})
            if tok[0] == "e":
                d[("e", tok[1])] = tok
            else:
                d[("d", tok[1])] = tok

    def op(self, eng, fn, r=(), w=(), signal=True):
        for t in self._deps(r, w, eng):
            self._wait(eng, t)
        ins = fn(self.e[eng])
        self.ninst += 1
        idx = self.cnt[eng] + 1
        if signal:
            ins.then_inc(self.sem[eng], 1)
            self.cnt[eng] = idx
            self.pending[eng] = False
        else:
            self.pending[eng] = True
        self._record(("e", eng, idx), r, w)

    def dma(self, q, out, in_, r=(), w=()):
        for t in self._deps(r, w):
            self._wait(q, t)
        lo, n = (0, self.nsp) if q != "pool" else (self.nsp, len(self.dsem) - self.nsp)
        s = lo + self.dnext_q[q] % n
        self.dnext_q[q] += 1
        if self.dval[s] > 0:
            self._wait(q, ("d", s, self.dval[s]))
        ins = self.e[q].dma_start(out=out, in_=in_)
        self.ninst += 1
        self.dval[s] += 16
        ins.then_inc(self.dsem[s], 16)
        self._record(("d", s, self.dval[s]), r, w)

    def barrier(self):
        self.bmarks.append(dict(self.cnt))
        for k in self.ENG:
            assert not self.pending[k], k
        for eng in self.ENG:
            for e2 in self.ENG:
                if e2 != eng and self.cnt[e2] > 0:
                    self._wait(eng, ("e", e2, self.cnt[e2]))
            for s in range(len(self.dsem)):
                if self.dval[s] > 0:
                    self._wait(eng, ("d", s, self.dval[s]))
        self.lastw = {}
        self.readers = {}


def build(stage=99, debug=False):
    nc = bass.Bass("TRN2", target_bir_lowering=False)
    dt = nc.dram_tensor
    xb = dt("xb", [S, D], F32, kind="ExternalInput").ap()
    xo = dt("xo", [TOK + HALO, D], F32, kind="ExternalInput").ap()
    posb = dt("posb", [1, S], I32, kind="ExternalInput").ap()
    poso = dt("poso", [1, TOK], I32, kind="ExternalInput").ap()
    po = dt("po", [TOK, 256], F32, kind="ExternalInput").ap()
    maskd = dt("mask", [128, 512], F32, kind="ExternalInput").ap()
    pcols_d = dt("pcols", [128, NCOL], F32, kind="ExternalInput").ap()
    prow = dt("prow", [1, NROW], F32, kind="ExternalInput").ap()
    w_in = dt("w_in", [D, 3904], F32, kind="ExternalInput").ap()
    w_uq = dt("w_uq", [512, H * 192], F32, kind="ExternalInput").ap()
    w_ukv = dt("w_ukv", [256, H * 256], F32, kind="ExternalInput").ap()
    w_out = dt("w_out", [D, D], F32, kind="ExternalInput").ap()
    w_r = dt("w_r", [D, 36], F32, kind="ExternalInput").ap()
    nee = NE if stage > 4 else 1
    w1 = dt("w1", [nee, D, DE], F32, kind="ExternalInput").ap()
    w3 = dt("w3", [nee, D, DE], F32, kind="ExternalInput").ap()
    w2 = dt("w2", [nee, DE, D], F32, kind="ExternalInput").ap()
    w_ple = dt("w_ple", [256, D], F32, kind="ExternalInput").ap()
    w_pg = dt("w_pg", [D, D], F32, kind="ExternalInput").ap()
    out_d = dt("out", [TOK, D], F32, kind="ExternalOutput").ap()
    dbg = {}
    if debug:
        dbg["kT"] = dt("dbg_kT", [192, S], F32, kind="ExternalOutput").ap()
        dbg["qT"] = dt("dbg_qT", [192, TOK], F32, kind="ExternalOutput").ap()
        dbg["v"] = dt("dbg_v", [128, 32 * 129], F32, kind="ExternalOutput").ap()
        dbg["yattn"] = dt("dbg_yattn", [TOK, 1024], F32, kind="ExternalOutput").ap()
        dbg["kvnT"] = dt("dbg_kvnT", [128, 2 * S], F32, kind="ExternalOutput").ap()
        dbg["h1"] = dt("dbg_h1", [TOK, D], F32, kind="ExternalOutput").ap()

    w_in_v = w_in.rearrange("(c p) n -> p c n", p=128)

    with ExitStack() as es:
        P = Prog(nc, es)
        ps = [nc.alloc_psum_tensor(f"ps{i}", [128, 512], F32).ap() for i in range(8)]
        psb = [p_.bitcast(BF16) for p_ in ps]

        ident = P.alloc("ident", [128, 128], BF16)
        ones = P.alloc("ones", [128, 128], BF16)
        onesf = P.alloc("onesf", [128, 1], F32)
        pcols = P.alloc("pcols", [128, NCOL], F32)
        stats = P.alloc("stats", [128, 512], F32)
        junk = P.alloc("junk", [128, D], BF16)
        stat_i = [0]

        P.op("pool", lambda e: e.memset(ident, 1.0), w=["ident"])
        P.op("pool", lambda e: e.affine_select(out=ident, in_=ident, pattern=[[-1, 128]], compare_op=ALU.is_equal,
                                               fill=0.0, base=0, channel_multiplier=1), r=["ident"], w=["ident"])
        P.op("pool", lambda e: e.memset(ones, 1.0), w=["ones"])
        P.op("pool", lambda e: e.memset(onesf, 1.0), w=["onesf"])
        P.op("pool", lambda e: e.memset(stats, 0.0), w=["stats"])
        P.dma("sp", pcols, pcols_d, w=["pcols"])

        def col(c, rows=128):
            return pcols[0:rows, c:c + 1]

        def newstat(n=1):
            i = stat_i[0]
            stat_i[0] += n
            assert stat_i[0] <= 512
            return i

        def rstd_from_psum(pss, rows, n, inv_n, tmp, dst, rkeys, tkey, dkey):
            P.op("act", lambda e: e.activation(out=tmp[0:rows, 0:n], in_=pss, func=AF.Ln, scale=inv_n, bias=col(C_PHASE + 1, rows)),
                 r=rkeys + ["pcols"], w=[tkey])
            P.op("act", lambda e: e.activation(out=dst[0:rows, 0:n], in_=tmp[0:rows, 0:n], func=AF.Exp, scale=-0.5), r=[tkey], w=[dkey])

        def rmsnorm_T(src, rows, gbc, gkey, dstT, col0, skey, dkey, xs, xskey, banks, nfeat=D, evac=("act", "dve")):
            si = newstat(3)
            ssq = stats[0:rows, si:si + 1]
            sq = stats[0:rows, si + 1:si + 2]
            rs = stats[0:rows, si + 2:si + 3]
            P.op("act", lambda e: e.activation(out=junk[0:rows, 0:nfeat], in_=src, func=AF.Square, accum_out=ssq),
                 r=[skey, "stats"], w=[("st", si)])
            P.op("act", lambda e: e.activation(out=sq, in_=ssq, func=AF.Sqrt, scale=1.0 / nfeat, bias=col(C_PHASE + 1, rows)),
                 r=[("st", si), "pcols"], w=[("st", si + 1)])
            P.op("dve", lambda e: e.reciprocal(out=rs, in_=sq), r=[("st", si + 1)], w=[("st", si + 2)])
            P.op("dve", lambda e: e.scalar_tensor_tensor(out=xs[0:rows, 0:nfeat], in0=src, scalar=rs, in1=gbc[0:rows, 0:nfeat],
                                                         op0=ALU.mult, op1=ALU.mult),
                 r=[skey, ("st", si + 2), gkey], w=[xskey])
            nch = nfeat // 128
            for hf in range(nch // 8):
                bk = banks[hf % len(banks)]
                for c8 in range(8):
                    c = hf * 8 + c8
                    P.op("pe", lambda e: e.transpose(out=psb[bk][:, c8 * 128:c8 * 128 + rows], in_=xs[0:rows, c * 128:(c + 1) * 128],
                                                     identity=ident[0:rows, 0:rows]),
                         r=[xskey, "ident"], w=[("ps", bk)], signal=(c8 == 7))
                src_v = psb[bk].rearrange("p (c t) -> p c t", c=8)[:, :, 0:rows]
                dst_v = dstT[:, hf * 8:(hf + 1) * 8, col0:col0 + rows]
                if evac[hf % 2] == "act":
                    P.op("act", lambda e: e.copy(out=dst_v, in_=src_v), r=[("ps", bk)], w=[dkey])
                else:
                    P.op(evac[hf % 2], lambda e: e.tensor_copy(out=dst_v, in_=src_v), r=[("ps", bk)], w=[dkey])

        def rope_tables(pos_dram, n0, n, tab, tkey, tmpf, tmpi, tmpi2, q="sp", t0=0):
            ti = tmpi[0:64, 0:n]
            P.dma(q, ti, pos_dram[0:1, n0:n0 + n].partition_broadcast(64), w=[tkey + "_i"])
            pf = tmpf[0:64, 0, 0:n]
            P.op("dve", lambda e: e.tensor_copy(out=pf, in_=ti), r=[tkey + "_i"], w=[tkey + "_pf"])
            ang = tab[0:64, :, t0:t0 + n]
            P.op("dve", lambda e: e.tensor_scalar(out=tab[0:64, 0, t0:t0 + n], in0=pf, scalar1=col(C_FREQ, 64), scalar2=math.pi / 2,
                                                   op0=ALU.mult, op1=ALU.add), r=[tkey + "_pf", "pcols"], w=[tkey])
            P.op("dve", lambda e: e.tensor_scalar(out=tab[0:64, 1, t0:t0 + n], in0=pf, scalar1=col(C_FREQ, 64), scalar2=None,
                                                   op0=ALU.mult), r=[tkey + "_pf", "pcols", tkey], w=[tkey])
            kf = tmpf[0:64, :, 0:n]
            P.op("dve", lambda e: e.tensor_scalar(out=kf, in0=ang, scalar1=1.0 / TWO_PI, scalar2=None, op0=ALU.mult),
                 r=[tkey], w=[tkey + "_pf"])
            kiv = tmpi2[0:64, :, 0:n]
            P.op("dve", lambda e: e.tensor_copy(out=kiv, in_=kf), r=[tkey + "_pf"], w=[tkey + "_ki"])
            P.op("dve", lambda e: e.tensor_copy(out=kf, in_=kiv), r=[tkey + "_ki"], w=[tkey + "_pf"])
            P.op("dve", lambda e: e.tensor_scalar(out=kf, in0=kf, scalar1=-TWO_PI, scalar2=None, op0=ALU.mult),
                 r=[tkey + "_pf"], w=[tkey + "_pf"])
            P.op("dve", lambda e: e.tensor_tensor(out=ang, in0=ang, in1=kf, op=ALU.add), r=[tkey + "_pf", tkey], w=[tkey])
            P.op("dve", lambda e: e.tensor_scalar(out=kf, in0=ang, scalar1=math.pi, scalar2=-TWO_PI, op0=ALU.is_gt, op1=ALU.mult),
                 r=[tkey], w=[tkey + "_pf"])
            P.op("dve", lambda e: e.tensor_tensor(out=ang, in0=ang, in1=kf, op=ALU.add), r=[tkey, tkey + "_pf"], w=[tkey])
            P.op("dve", lambda e: e.tensor_scalar(out=kf, in0=ang, scalar1=-math.pi, scalar2=TWO_PI, op0=ALU.is_lt, op1=ALU.mult),
                 r=[tkey], w=[tkey + "_pf"])
            P.op("dve", lambda e: e.tensor_tensor(out=ang, in0=ang, in1=kf, op=ALU.add), r=[tkey, tkey + "_pf"], w=[tkey])
            P.op("dve", lambda e: e.tensor_scalar(out=ang, in0=ang, scalar1=3.1415925, scalar2=-3.1415925, op0=ALU.min, op1=ALU.max),
                 r=[tkey], w=[tkey])
            P.op("act", lambda e: e.activation(out=tab[0:64, 0, t0:t0 + n], in_=tab[0:64, 0, t0:t0 + n], func=AF.Sin), r=[tkey], w=[tkey])
            P.op("act", lambda e: e.activation(out=tab[0:64, 1, t0:t0 + n], in_=tab[0:64, 1, t0:t0 + n], func=AF.Sin, scale=col(C_SGN, 64)),
                 r=[tkey, "pcols"], w=[tkey])

        m_persist = P.mark()
        ycgT = P.alloc("ycgT", [128, 8, TOK], BF16)
        rsconv = P.alloc("rsconv", [128, 8], F32)
        yanT = P.alloc("yanT", [128, 8, TOK], BF16)
        m_keep = P.mark()
        kvnT = P.alloc("kvnT", [128, 2, S], BF16)
        ropeK = P.alloc("ropeK", [64, S], BF16)
        sqkr = P.alloc("sqkr", [64, S], BF16)
        qnT = P.alloc("qnT", [128, 4, TOK], BF16)
        tabq = P.alloc("tabq", [64, 2, TOK], F32)
        m_ab = P.mark()

        gbc = P.alloc("gmix_bc", [128, D], F32)
        wkv = P.alloc("wkv", [128, 16, 384], BF16)
        xblk = [P.alloc(f"xblk{i}", [128, D], F32) for i in range(2)]
        xs = [P.alloc(f"xs{i}", [128, D], BF16) for i in range(2)]
        xnT = [P.alloc(f"xnT{i}", [128, 16, 512], BF16) for i in range(2)]
        sqt = [P.alloc(f"sqt{i}", [128, 2, 512], BF16) for i in range(2)]
        rbc = [P.alloc(f"rbc{i}", [128, 512], F32) for i in range(2)]
        rtmp = P.alloc("rtmp", [128, 512], F32)
        tabk = [P.alloc(f"tabk{i}", [64, 2, 512], F32) for i in range(2)]
        tmpf = P.alloc("tmpf", [64, 2, 512], F32)
        tmpi = P.alloc("tmpi", [64, 512], I32)
        tmpi2 = P.alloc("tmpi2", [64, 2, 512], I32)
        t1 = P.alloc("t1", [64, 512], F32)
        t2 = P.alloc("t2", [64, 512], F32)

        P.dma("sp", gbc, prow[0:1, R_NMIX:R_NMIX + D].partition_broadcast(128), w=["gbc"])
        P.dma("pool", wkv[:, :, 0:256], w_in_v[:, :, 512:768], w=["wkv"])
        P.dma("pool", wkv[:, :, 256:320], w_in_v[:, :, 768:832], w=["wkv"])
        P.dma("pool", wkv[:, :, 320:352], w_in_v[:, :, 800:832], w=["wkv"])
        P.dma("pool", wkv[:, :, 352:384], w_in_v[:, :, 768:800], w=["wkv"])

        for hq in range(2):
            rope_tables(poso, hq * 512, 512, tabq, "tabq", tmpf, tmpi, tmpi2, t0=hq * 512)
        NG = S // 512
        def emit_blocks(g, bls, rope):
            xg_ = xnT[g % 2]
            for bl in bls:
                t = g * 4 + bl
                xb_t = xblk[t % 2]
                P.dma("sp", xb_t, xb[t * 128:(t + 1) * 128, :], w=[("xblk", t % 2)])
                rmsnorm_T(xb_t, 128, gbc, "gbc", xg_, bl * 128, ("xblk", t % 2), ("xnT", g % 2), xs[t % 2], ("xs", t % 2), banks=[0, 1])
            if rope:
                rope_tables(posb, g * 512, 512, tabk[g % 2], f"tabk{g % 2}", tmpf, tmpi, tmpi2)

        emit_blocks(0, (0, 1), False)
        for g in range(NG):
            emit_blocks(g, (2, 3), True)
            xg = xnT[g % 2]
            xgk = ("xnT", g % 2)
            tb = tabk[g % 2]
            tbk = f"tabk{g % 2}"
            tiles = [(2, 0, 128), (3, 128, 128), (4, 256, 64), (5, 320, 64)]
            for (bk, c0, m) in tiles:
                for c in range(16):
                    P.op("pe", lambda e: e.matmul(ps[bk][0:m, :], lhsT=wkv[:, c, c0:c0 + m], rhs=xg[:, c, :], start=(c == 0), stop=(c == 15)),
                         r=["wkv", xgk], w=[("ps", bk)], signal=(c == 15))
            if g + 1 < NG:
                emit_blocks(g + 1, (0, 1), False)
            sq = sqt[g % 2]
            sqk = ("sqt", g % 2)
            for j in range(2):
                P.op("act", lambda e: e.activation(out=sq[:, j, :], in_=ps[2 + j], func=AF.Square), r=[("ps", 2 + j)], w=[sqk])
            for j in range(2):
                P.op("pe", lambda e: e.matmul(ps[6], lhsT=ones, rhs=sq[:, j, :], start=(j == 0), stop=(j == 1)),
                     r=["ones", sqk], w=[("ps", 6)], signal=(j == 1))
            rb = rbc[g % 2]
            rbk = ("rbc", g % 2)
            rstd_from_psum(ps[6], 128, 512, 1.0 / 256, rtmp, rb, [("ps", 6)], "rtmp", rbk)
            for j in range(2):
                P.op("dve", lambda e: e.scalar_tensor_tensor(out=kvnT[:, j, g * 512:(g + 1) * 512], in0=ps[2 + j], scalar=col(C_GKV + j),
                                                             in1=rb, op0=ALU.mult, op1=ALU.mult),
                     r=[("ps", 2 + j), rbk, "pcols"], w=["kvnT"])
            P.op("act", lambda e: e.activation(out=sqkr[:, g * 512:(g + 1) * 512], in_=ps[4][0:64, :], func=AF.Square),
                 r=[("ps", 4)], w=["sqkr"])
            P.op("dve", lambda e: e.scalar_tensor_tensor(out=t1, in0=ps[4][0:64, :], scalar=col(C_GKR, 64), in1=tb[0:64, 0, :],
                                                         op0=ALU.mult, op1=ALU.mult), r=[("ps", 4), tbk, "pcols"], w=["t1"])
            P.op("dve", lambda e: e.scalar_tensor_tensor(out=t2, in0=ps[5][0:64, :], scalar=col(C_GKRS, 64), in1=tb[0:64, 1, :],
                                                         op0=ALU.mult, op1=ALU.mult), r=[("ps", 5), tbk, "pcols"], w=["t2"])
            P.op("dve", lambda e: e.tensor_tensor(out=ropeK[:, g * 512:(g + 1) * 512], in0=t1, in1=t2, op=ALU.add),
                 r=["t1", "t2"], w=["ropeK"])

        if stage <= 1:
            return _finish(nc, P, out_d)

        P.barrier()
        P.release(m_ab)
        xnTo = P.alloc("xnTo", [128, 16, TOK + HALO], BF16)
        m_b1 = P.mark()
        gbc = P.alloc("gmix_bc", [128, D], F32)
        xblk = [P.alloc(f"xblk{i}", [128, D], F32) for i in range(2)]
        xs = [P.alloc(f"xs{i}", [128, D], BF16) for i in range(2)]
        P.dma("sp", gbc, prow[0:1, R_NMIX:R_NMIX + D].partition_broadcast(128), w=["gbc"])
        for bl in range(NB_OWN + 1):
            rows = 128 if bl < NB_OWN else HALO
            xb_t = xblk[bl % 2]
            P.dma("sp", xb_t[0:rows, :], xo[bl * 128:bl * 128 + rows, :], w=[("xblk", bl % 2)])
            rmsnorm_T(xb_t[0:rows, :], rows, gbc, "gbc", xnTo, bl * 128, ("xblk", bl % 2), "xnTo", xs[bl % 2], ("xs", bl % 2), banks=[6, 7])
        P.barrier()
        P.release(m_b1)
        wq = P.alloc("wq", [128, 16, 512], BF16)
        wcv = [P.alloc(f"wcv{i}", [128, 3, 16, 128], BF16) for i in range(2)]
        sq4 = P.alloc("sq4", [128, 4, 512], BF16)
        rb = P.alloc("rbq", [128, 512], F32)
        rtmp = P.alloc("rtmpq", [128, 512], F32)
        cs = [P.alloc(f"cs{i}", [128, 512], F32) for i in range(2)]
        uu = [P.alloc(f"uu{i}", [128, 8, 130], F32) for i in range(2)]
        acc = [P.alloc(f"acc{i}", [128, 512], F32) for i in range(2)]
        yc = [P.alloc(f"yc{i}", [128, 512], F32) for i in range(2)]
        sqsum = P.alloc("sqsum", [128, TOK], F32)
        sqc = P.alloc("sqc", [128, 512], F32)
        hal = P.alloc("hal", [128, 32], F32)

        P.dma("pool", wq, w_in_v[:, :, 0:512], w=["wq"])
        P.op("pool", lambda e: e.memset(sqsum, 0.0), w=["sqsum"])
        for tg in range(2):
            for mt in range(4):
                for c in range(16):
                    P.op("pe", lambda e: e.matmul(ps[mt], lhsT=wq[:, c, mt * 128:(mt + 1) * 128], rhs=xnTo[:, c, tg * 512:(tg + 1) * 512],
                                                  start=(c == 0), stop=(c == 15)), r=["wq", "xnTo"], w=[("ps", mt)], signal=(c == 15))
                P.op("act", lambda e: e.activation(out=sq4[:, mt, :], in_=ps[mt], func=AF.Square), r=[("ps", mt)], w=["sq4"])
            for mt in range(4):
                P.op("pe", lambda e: e.matmul(ps[4], lhsT=ones, rhs=sq4[:, mt, :], start=(mt == 0), stop=(mt == 3)),
                     r=["ones", "sq4"], w=[("ps", 4)], signal=(mt == 3))
            rstd_from_psum(ps[4], 128, 512, 1.0 / 512, rtmp, rb, [("ps", 4)], "rtmpq", "rbq")
            for mt in range(4):
                P.op("dve", lambda e: e.scalar_tensor_tensor(out=qnT[:, mt, tg * 512:(tg + 1) * 512], in0=ps[mt], scalar=col(C_GQL + mt),
                                                             in1=rb, op0=ALU.mult, op1=ALU.mult),
                     r=[("ps", mt), "rbq", "pcols"], w=["qnT"])
        for ct in range(8):
            wv = wcv[ct % 2]
            wk = ("wcv", ct % 2)
            for gi, base in enumerate((832, 1856, 2880)):
                P.dma("pool", wv[:, gi, :, :], w_in_v[:, :, base + ct * 128: base + (ct + 1) * 128], w=[wk])
            for gi, c0 in ((1, 0), (2, 16)):
                for c in range(16):
                    P.op("pe", lambda e: e.matmul(ps[6][:, c0:c0 + 16], lhsT=wv[:, gi, c, :], rhs=xnTo[:, c, TOK:TOK + HALO],
                                                  start=(c == 0), stop=(c == 15)), r=[wk, "xnTo"], w=[("ps", 6)], signal=(c == 15))
            P.op("act", lambda e: e.copy(out=hal, in_=ps[6][:, 0:32]), r=[("ps", 6)], w=["hal"])
            for seg in range(2):
                par = (ct * 2 + seg) % 2
                bks = (0, 1, 2) if par == 0 else (3, 4, 5)
                u = uu[par]
                uk = ("uu", par)
                P.op("dve", lambda e: e.tensor_tensor(out=u[:, seg * 4:(seg + 1) * 4, 0:2],
                                                       in0=hal[:, 0:16].rearrange("p (b t) -> p b t", t=2)[:, seg * 4:(seg + 1) * 4, :],
                                                       in1=hal[:, 16:32].rearrange("p (b t) -> p b t", t=2)[:, seg * 4:(seg + 1) * 4, :],
                                                       op=ALU.mult), r=["hal"], w=[uk])
                for gi, bk in ((1, bks[0]), (2, bks[1]), (0, bks[2])):
                    for c in range(16):
                        P.op("pe", lambda e: e.matmul(ps[bk], lhsT=wv[:, gi, c, :], rhs=xnTo[:, c, seg * 512:(seg + 1) * 512],
                                                      start=(c == 0), stop=(c == 15)), r=[wk, "xnTo"], w=[("ps", bk)], signal=(c == 15))
                P.op("act", lambda e: e.copy(out=cs[par], in_=ps[bks[0]]), r=[("ps", bks[0])], w=[("cs", par)])
                P.op("dve", lambda e: e.tensor_tensor(out=u[:, seg * 4:(seg + 1) * 4, 2:130], in0=cs[par].rearrange("p (b t) -> p b t", t=128),
                                                      in1=ps[bks[1]].rearrange("p (b t) -> p b t", t=128), op=ALU.mult),
                     r=[("cs", par), ("ps", bks[1])], w=[uk])
                a = acc[par].rearrange("p (b t) -> p b t", t=128)
                ak = ("acc", par)
                us = u[:, seg * 4:(seg + 1) * 4, :]
                P.op("act", lambda e: e.activation(out=a, in_=us[:, :, 2:130], func=AF.Copy, scale=col(C_WCONV + ct * 3 + 2)),
                     r=[uk, "pcols"], w=[ak])
                P.op("dve", lambda e: e.scalar_tensor_tensor(out=a, in0=us[:, :, 1:129], scalar=col(C_WCONV + ct * 3 + 1), in1=a,
                                                             op0=ALU.mult, op1=ALU.add), r=[uk, "pcols", ak], w=[ak])
                P.op("dve", lambda e: e.scalar_tensor_tensor(out=a, in0=us[:, :, 0:128], scalar=col(C_WCONV + ct * 3 + 0), in1=a,
                                                             op0=ALU.mult, op1=ALU.add), r=[uk, "pcols", ak], w=[ak])
                P.op("dve", lambda e: e.tensor_tensor(out=yc[par], in0=acc[par], in1=ps[bks[2]], op=ALU.mult),
                     r=[ak, ("ps", bks[2])], w=[("yc", par)])
                P.op("act", lambda e: e.activation(out=ycgT[:, ct, seg * 512:(seg + 1) * 512], in_=yc[par], func=AF.Copy, scale=col(C_GOC + ct)),
                     r=[("yc", par), "pcols"], w=["ycgT"])
                P.op("act", lambda e: e.activation(out=sqc, in_=yc[par], func=AF.Square), r=[("yc", par)], w=["sqc"])
                P.op("dve", lambda e: e.tensor_tensor(out=sqsum[:, seg * 512:(seg + 1) * 512], in0=sqsum[:, seg * 512:(seg + 1) * 512],
                                                       in1=sqc, op=ALU.add), r=["sqc", "sqsum"], w=["sqsum"])
        for bl in range(8):
            P.op("pe", lambda e: e.matmul(ps[7][:, bl:bl + 1], lhsT=sqsum[:, bl * 128:(bl + 1) * 128], rhs=onesf, start=True, stop=True),
                 r=["sqsum", "onesf"], w=[("ps", 7)], signal=(bl == 7))
        si = newstat(8)
        rstd_from_psum(ps[7][:, 0:8], 128, 8, 1.0 / 1024, stats[:, si:si + 8], rsconv, [("ps", 7)], ("st", si), "rsconv")

        if stage <= 2:
            return _finish(nc, P, out_d)
        P.barrier()
        P.release(m_ab)
        yattn = P.alloc("yattn", [128, 8, 1024], F32)
        goa = P.alloc("goa_bc", [128, 1024], F32)
        xsd = [P.alloc(f"xsd{i}", [128, D], BF16) for i in range(2)]
        P.dma("sp", goa, prow[0:1, R_GOA:R_GOA + 1024].partition_broadcast(128), w=["goa"])
        wukv = P.alloc("wukv", [128, 2, H * 256], BF16)
        wuq = P.alloc("wuq", [128, 4, H * 256], BF16)
        KnT = P.alloc("KnT", [128, S], BF16)
        KrT = P.alloc("KrT", [64, S], BF16)
        V1 = P.alloc("V1", [128, 32, 129], BF16)
        QnT = P.alloc("QnT", [128, TOK], BF16)
        QrT = P.alloc("QrT", [64, TOK], BF16)
        PT = [P.alloc(f"PT{i}", [128, TOK], BF16) for i in range(3)]
        mask = P.alloc("mask", [128, 512], BF16)
        sqk = [P.alloc(f"sqk{i}", [128, 512], BF16) for i in range(2)]
        sqr = [P.alloc(f"sqr{i}", [64, 512], BF16) for i in range(2)]
        rb2 = [P.alloc(f"rb2{i}", [128, 512], F32) for i in range(2)]
        rt2 = [P.alloc(f"rt2{i}", [128, 512], F32) for i in range(2)]
        q1 = P.alloc("q1", [64, 512], F32)
        q2 = P.alloc("q2", [64, 512], F32)
        rinv = P.alloc("rinv", [128, 8], F32)

        P.dma("pool", mask, maskd, w=["mask"])
        w_ukv_v = w_ukv.rearrange("(c p) n -> p c n", p=128)
        wuq_v = w_uq.rearrange("(c p) (h d) -> p c h d", p=128, h=H)
        wuq_h = wuq.rearrange("p c (h d) -> p c h d", h=H)
        P.op("pool", lambda e: e.memset(V1[:, :, 128:129], 1.0), w=["V1"])

        _save_c = P.mark()
        P.release(m_keep)
        h1 = P.alloc("h1", [128, 8, D], F32)
        hnT = P.alloc("hnT", [128, 16, TOK], BF16)
        m_d = P.mark()
        wo = [P.alloc(f"wo{i}", [128, 16, 512], BF16) for i in range(2)]
        m_wo_end = P.mark()
        P.release(_save_c)
        w_out_v = w_out.rearrange("(c p) n -> p c n", p=128)
        for h in range(H):
            P.hmarks.append(('build', h, dict(P.cnt)))
            if os.environ.get('K_F1', '1') == '1':
                P.dma("pool", wukv[:, :, h * 256:(h + 1) * 256], w_ukv_v[:, :, h * 256:(h + 1) * 256], w=["wukv"])
                for c4 in range(4):
                    P.dma("pool", wuq_h[:, c4, h, 0:192], wuq_v[:, c4, h, :], w=["wuq"])
                    P.dma("pool", wuq_h[:, c4, h, 192:224], wuq_v[:, c4, h, 160:192], w=["wuq"])
                    P.dma("pool", wuq_h[:, c4, h, 224:256], wuq_v[:, c4, h, 128:160], w=["wuq"])
            elif h == 0:
                P.dma("pool", wukv, w_ukv_v, w=["wukv"])
                for c4 in range(4):
                    P.dma("pool", wuq_h[:, c4, :, 0:192], wuq_v[:, c4, :, :], w=["wuq"])
                    P.dma("pool", wuq_h[:, c4, :, 192:224], wuq_v[:, c4, :, 160:192], w=["wuq"])
                    P.dma("pool", wuq_h[:, c4, :, 224:256], wuq_v[:, c4, :, 128:160], w=["wuq"])
            for g in range(NG):
                bk = g % 2
                bs = 2 + g % 2
                for j in range(2):
                    P.op("pe", lambda e: e.matmul(ps[bk], lhsT=wukv[:, j, h * 256:h * 256 + 128], rhs=kvnT[:, j, g * 512:(g + 1) * 512],
                                                  start=(j == 0), stop=(j == 1)), r=["wukv", "kvnT"], w=[("ps", bk)], signal=(j == 1))
                sk = sqk[g % 2]
                P.op("act", lambda e: e.activation(out=sk, in_=ps[bk], func=AF.Square), r=[("ps", bk)], w=[("sqk", g % 2)])
                P.op("pe", lambda e: e.matmul(ps[bs], lhsT=ones, rhs=sk, start=True, stop=False), r=["ones", ("sqk", g % 2)], w=[("ps", bs)], signal=False)
                P.op("pe", lambda e: e.matmul(ps[bs], lhsT=ones[0:64, :], rhs=sqkr[:, g * 512:(g + 1) * 512], start=False, stop=True),
                     r=["ones", "sqkr"], w=[("ps", bs)])
                rstd_from_psum(ps[bs], 128, 512, 1.0 / 192, rt2[g % 2], rb2[g % 2], [("ps", bs)], ("rt2", g % 2), ("rb2", g % 2))
                P.op("dve", lambda e: e.scalar_tensor_tensor(out=KnT[:, g * 512:(g + 1) * 512], in0=ps[bk], scalar=col(C_GKN), in1=rb2[g % 2],
                                                             op0=ALU.mult, op1=ALU.mult), r=[("ps", bk), ("rb2", g % 2), "pcols"], w=["KnT"])
                P.op("dve", lambda e: e.tensor_tensor(out=KrT[:, g * 512:(g + 1) * 512], in0=ropeK[:, g * 512:(g + 1) * 512],
                                                       in1=rb2[g % 2][0:64, :], op=ALU.mult), r=["ropeK", ("rb2", g % 2)], w=["KrT"])
                vb = 4
                for kb4 in range(4):
                    kb = g * 4 + kb4
                    for j in range(2):
                        P.op("pe", lambda e: e.matmul(ps[vb][:, kb4 * 128:(kb4 + 1) * 128], lhsT=kvnT[:, j, kb * 128:(kb + 1) * 128],
                                                      rhs=wukv[:, j, h * 256 + 128:h * 256 + 256], start=(j == 0), stop=(j == 1)),
                             r=["wukv", "kvnT"], w=[("ps", vb)], signal=(j == 1 and kb4 == 3))
                P.op("act", lambda e: e.copy(out=V1[:, g * 4:(g + 1) * 4, 0:128], in_=ps[vb].rearrange("p (b d) -> p b d", b=4)),
                     r=[("ps", vb)], w=["V1"])
            for tg in range(2):
                tsl = slice(tg * 512, (tg + 1) * 512)
                bq = 0
                for j in range(4):
                    P.op("pe", lambda e: e.matmul(ps[bq], lhsT=wuq[:, j, h * 256:h * 256 + 128], rhs=qnT[:, j, tsl], start=(j == 0), stop=(j == 3)),
                         r=["wuq", "qnT"], w=[("ps", bq)], signal=(j == 3))
                for j in range(4):
                    P.op("pe", lambda e: e.matmul(ps[1][0:64, :], lhsT=wuq[:, j, h * 256 + 128:h * 256 + 192], rhs=qnT[:, j, tsl], start=(j == 0), stop=(j == 3)),
                         r=["wuq", "qnT"], w=[("ps", 1)], signal=(j == 3))
                for j in range(4):
                    P.op("pe", lambda e: e.matmul(ps[4][0:64, :], lhsT=wuq[:, j, h * 256 + 192:h * 256 + 256], rhs=qnT[:, j, tsl], start=(j == 0), stop=(j == 3)),
                         r=["wuq", "qnT"], w=[("ps", 4)], signal=(j == 3))
                P.op("act", lambda e: e.activation(out=sqk[tg], in_=ps[bq], func=AF.Square), r=[("ps", bq)], w=[("sqk", tg)])
                P.op("act", lambda e: e.activation(out=sqr[tg], in_=ps[1][0:64, :], func=AF.Square), r=[("ps", 1)], w=[("sqr", tg)])
                bs = 2 + tg
                P.op("pe", lambda e: e.matmul(ps[bs], lhsT=ones, rhs=sqk[tg], start=True, stop=False), r=["ones", ("sqk", tg)], w=[("ps", bs)], signal=False)
                P.op("pe", lambda e: e.matmul(ps[bs], lhsT=ones[0:64, :], rhs=sqr[tg], start=False, stop=True), r=["ones", ("sqr", tg)], w=[("ps", bs)])
                rstd_from_psum(ps[bs], 128, 512, 1.0 / 192, rt2[tg], rb2[tg], [("ps", bs)], ("rt2", tg), ("rb2", tg))
                P.op("dve", lambda e: e.scalar_tensor_tensor(out=QnT[:, tsl], in0=ps[bq], scalar=col(C_GQN), in1=rb2[tg], op0=ALU.mult, op1=ALU.mult),
                     r=[("ps", bq), ("rb2", tg), "pcols"], w=["QnT"])
                P.op("dve", lambda e: e.scalar_tensor_tensor(out=q1, in0=ps[1][0:64, :], scalar=col(C_GQR, 64), in1=tabq[0:64, 0, tsl],
                                                             op0=ALU.mult, op1=ALU.mult), r=[("ps", 1), "tabq", "pcols"], w=["q1"])
                P.op("dve", lambda e: e.scalar_tensor_tensor(out=q2, in0=ps[4][0:64, :], scalar=col(C_GQRS, 64), in1=tabq[0:64, 1, tsl],
                                                             op0=ALU.mult, op1=ALU.mult), r=[("ps", 4), "tabq", "pcols"], w=["q2"])
                P.op("dve", lambda e: e.tensor_tensor(out=q1, in0=q1, in1=q2, op=ALU.add), r=["q1", "q2"], w=["q1"])
                P.op("dve", lambda e: e.tensor_tensor(out=QrT[:, tsl], in0=q1, in1=rb2[tg][0:64, :], op=ALU.mult), r=["q1", ("rb2", tg)], w=["QrT"])
            P.hmarks.append(('attn', h, dict(P.cnt)))
            if h == H - 1:
                dead = ["kvnT", "ropeK", "sqkr", "qnT", "tabq"]
                for bl in range(6):
                    P.dma("sp", h1[:, bl, :], xo[bl * 128:(bl + 1) * 128, :], w=[("h1", bl)] + dead)
                P.dma("pool", wo[0][:, 0:8, :], w_out_v[:, 0:8, 0:512], w=[("wo", 0), "wukv", "wuq"])
                P.dma("pool", wo[0][:, 8:16, :], w_out_v[:, 8:16, 0:512], w=[("wo", 0), "wukv", "wuq"])
            for bk in (5, 6, 7):
                P.op("dve", lambda e: e.memset(ps[bk], 0.0), w=[("ps", bk)])
            sbank = [0]

            def emit_S(kb):
                i0_ = kb // 4
                q0_ = i0_ * 128
                chunks = [(q0_, min(q0_ + 512, TOK))]
                if chunks[0][1] < TOK:
                    chunks.append((chunks[0][1], TOK))
                res_ = []
                for (a0, a1) in chunks:
                    sb_ = sbank[0] % 4
                    sbank[0] += 1
                    n = a1 - a0
                    P.op("pe", lambda e: e.matmul(ps[sb_][:, 0:n], lhsT=KnT[:, kb * 128:(kb + 1) * 128], rhs=QnT[:, a0:a1], start=True, stop=False),
                         r=["KnT", "QnT"], w=[("ps", sb_)], signal=False)
                    P.op("pe", lambda e: e.matmul(ps[sb_][:, 0:n], lhsT=KrT[:, kb * 128:(kb + 1) * 128], rhs=QrT[:, a0:a1], start=False, stop=True),
                         r=["KrT", "QrT"], w=[("ps", sb_)])
                    res_.append((a0, a1, sb_))
                return res_

            for wi in range(N_WARM):
                P.op("pe", lambda e: e.matmul(ps[4], lhsT=ones, rhs=KnT[:, (wi % 8) * 512:(wi % 8 + 1) * 512], start=True, stop=True),
                     r=["ones", "KnT"], w=[("ps", 4)], signal=(wi == N_WARM - 1))
            cur = emit_S(0)
            for kb in range(32):
                i0 = kb // 4
                q0 = i0 * 128
                pt = PT[kb % 3]
                ptk = ("PT", kb % 3)
                for (a0, a1, sb_) in cur:
                    P.op("act", lambda e: e.activation(out=pt[:, a0:a1], in_=ps[sb_][:, 0:a1 - a0], func=AF.Exp, scale=SCALE), r=[("ps", sb_)], w=[ptk])
                P.op("dve", lambda e: e.tensor_tensor(out=pt[:, q0:q0 + 128], in0=pt[:, q0:q0 + 128], in1=mask[:, (kb % 4) * 128:(kb % 4 + 1) * 128],
                                                       op=ALU.mult), r=[ptk, "mask"], w=[ptk])
                if kb + 1 < 32:
                    cur = emit_S(kb + 1)
                nfill = N_FILL if kb < 16 else max(N_FILL - 1, 0)
                for wi in range(nfill):
                    P.op("pe", lambda e: e.matmul(ps[4], lhsT=ones, rhs=KnT[:, (wi % 8) * 512:(wi % 8 + 1) * 512], start=True, stop=True),
                         r=["ones", "KnT"], w=[("ps", 4)], signal=(wi == nfill - 1))
                for i in range(i0, 8):
                    ob = 5 + i // 3
                    oc = (i % 3) * 129
                    P.op("pe", lambda e: e.matmul(ps[ob][:, oc:oc + 129], lhsT=pt[:, i * 128:(i + 1) * 128], rhs=V1[:, kb, :], start=False, stop=False,
                                                  skip_group_check=True), r=[ptk, "V1", ("ps", ob)], w=[("pso", i)], signal=(i == 7))
                if kb % 4 == 3:
                    i = i0
                    ob = 5 + i // 3
                    oc = (i % 3) * 129
                    P.op("dve", lambda e: e.reciprocal(out=rinv[:, i:i + 1], in_=ps[ob][:, oc + 128:oc + 129]), r=[("pso", i), ("ps", ob)], w=[("rinv", i)])
                    P.op("act", lambda e: e.activation(out=yattn[:, i, h * 128:(h + 1) * 128], in_=ps[ob][:, oc:oc + 128], func=AF.Copy, scale=rinv[:, i:i + 1]),
                         r=[("pso", i), ("ps", ob), ("rinv", i)], w=[("yattn", i)])
        if debug:
            P.barrier()
            for i in range(8):
                P.dma("sp", dbg["yattn"][i * 128:(i + 1) * 128, :], yattn[:, i, :], r=[("yattn", i)])
        if stage <= 3:
            return _finish(nc, P, out_d)
        for i in range(8):
            rmsnorm_T(yattn[:, i, :], 128, goa, "goa", yanT, i * 128, ("yattn", i), "yanT", xsd[i % 2], ("xs", i % 2), banks=[0, 1], nfeat=1024)
        P.barrier()
        P.release(m_keep)
        P.off = m_wo_end
        gmoe = P.alloc("gmoe_bc", [128, D], F32)
        xs0 = P.alloc("xs_d0", [128, D], BF16)
        xs1 = P.alloc("xs_d1", [128, D], BF16)
        P.dma("sp", gmoe, prow[0:1, R_NMOE:R_NMOE + D].partition_broadcast(128), w=["gmoe"])
        for bl in range(6, 8):
            P.dma("sp", h1[:, bl, :], xo[bl * 128:(bl + 1) * 128, :], w=[("h1", bl)])
        for cg in range(4):
            wob = wo[cg % 2]
            wok = ("wo", cg % 2)
            if cg > 0:
                P.dma("pool", wob[:, 0:8, :], w_out_v[:, 0:8, cg * 512:(cg + 1) * 512], w=[wok])
                P.dma("pool", wob[:, 8:16, :], w_out_v[:, 8:16, cg * 512:(cg + 1) * 512], w=[wok])
            for bl in range(8):
                ba = (bl % 2) * 2
                bc = ba + 1
                for c in range(8):
                    P.op("pe", lambda e: e.matmul(ps[ba], lhsT=yanT[:, c, bl * 128:(bl + 1) * 128], rhs=wob[:, c, :], start=(c == 0), stop=(c == 7)),
                         r=["yanT", wok], w=[("ps", ba)], signal=(c == 7))
                for c in range(8):
                    P.op("pe", lambda e: e.matmul(ps[bc], lhsT=ycgT[:, c, bl * 128:(bl + 1) * 128], rhs=wob[:, 8 + c, :], start=(c == 0), stop=(c == 7)),
                         r=["ycgT", wok], w=[("ps", bc)], signal=(c == 7))
                hsl = h1[:, bl, cg * 512:(cg + 1) * 512]
                P.op("dve", lambda e: e.tensor_tensor(out=hsl, in0=ps[ba], in1=hsl, op=ALU.add), r=[("ps", ba), ("h1", bl)], w=[("h1", bl)])
                P.op("dve", lambda e: e.scalar_tensor_tensor(out=hsl, in0=ps[bc], scalar=rsconv[:, bl:bl + 1], in1=hsl, op0=ALU.mult, op1=ALU.add),
                     r=[("ps", bc), ("h1", bl), "rsconv"], w=[("h1", bl)])
        if debug:
            P.barrier()
            for bl in range(8):
                P.dma("sp", dbg["h1"][bl * 128:(bl + 1) * 128, :], h1[:, bl, :], r=[("h1", bl)])
        for bl in range(8):
            rmsnorm_T(h1[:, bl, :], 128, gmoe, "gmoe", hnT, bl * 128, ("h1", bl), "hnT", [xs0, xs1][bl % 2], ("xs", bl % 2), banks=[4, 5])
        if stage <= 4:
            for bl in range(8):
                P.dma("sp", out_d[bl * 128:(bl + 1) * 128, :], h1[:, bl, :], r=[("h1", bl)])
            return _finish(nc, P, out_d)
        P.barrier()
        P.release(m_d)
        wr = P.alloc("wr", [128, 16, 36], BF16)
        brb = P.alloc("brb", [128, 36], F32)
        comb = P.alloc("comb", [128, 8, 32], F32)
        rt = P.alloc("rt", [128, 256], F32)
        w13 = [P.alloc(f"w13_{i}", [128, 2, 16, 256], BF16) for i in range(3)]
        hid = [P.alloc(f"hid{i}", [128, 2, 4, 512], BF16) for i in range(2)]
        hi_save = P.mark()
        P.release(m_persist)
        w2b = [P.alloc(f"w2_{i}", [128, 2, D], BF16) for i in range(3)]
        sil = [P.alloc(f"sil{i}", [128, 512], F32) for i in range(2)]
        assert P.mark() <= m_keep
        P.release(hi_save)
        P.dma("pool", wr, w_r.rearrange("(c p) n -> p c n", p=128), w=["wr"])
        P.dma("sp", brb, prow[0:1, R_BR:R_BR + 36].partition_broadcast(128), w=["brb"])

        def rcol(i, n=1):
            return rt[:, i:i + n]

        for bl in range(8):
            for c in range(16):
                P.op("pe", lambda e: e.matmul(ps[7][:, bl * 36:(bl + 1) * 36], lhsT=hnT[:, c, bl * 128:(bl + 1) * 128], rhs=wr[:, c, :], start=(c == 0), stop=(c == 15)),
                     r=["hnT", "wr"], w=[("psr", bl)], signal=(c == 15))

        def route_block(bl):
            lg = rcol(0, 36)
            k = "rt"
            V = lambda f: P.op("dve", f, r=[k, "brb", ("psr", bl)], w=[k])
            P.op("dve", lambda e: e.tensor_tensor(out=lg, in0=ps[7][:, bl * 36:(bl + 1) * 36], in1=brb, op=ALU.add), r=[("psr", bl), "brb"], w=[k])
            gl = rcol(0, 4)
            el = rcol(4, 32)
            V(lambda e: e.tensor_reduce(out=rcol(40), in_=gl, axis=AX.X, op=ALU.max))
            V(lambda e: e.tensor_scalar(out=rcol(44, 4), in0=gl, scalar1=rcol(40), scalar2=None, op0=ALU.subtract))
            P.op("act", lambda e: e.activation(out=rcol(48, 4), in_=rcol(44, 4), func=AF.Exp, accum_out=rcol(41)), r=[k], w=[k])
            V(lambda e: e.reciprocal(out=rcol(42), in_=rcol(41)))
            V(lambda e: e.tensor_scalar(out=rcol(52, 4), in0=gl, scalar1=rcol(40), scalar2=None, op0=ALU.is_equal))
            V(lambda e: e.tensor_scalar(out=rcol(56, 4), in0=rcol(52, 4), scalar1=1.0, scalar2=BIG, op0=ALU.subtract, op1=ALU.mult))
            elm = rcol(64, 32)
            V(lambda e: e.tensor_tensor(out=elm.rearrange("p (g x) -> p g x", g=4), in0=el.rearrange("p (g x) -> p g x", g=4),
                                        in1=rcol(56, 4).unsqueeze(2).to_broadcast([128, 4, 8]), op=ALU.add))
            V(lambda e: e.tensor_reduce(out=rcol(96), in_=elm, axis=AX.X, op=ALU.max))
            V(lambda e: e.tensor_scalar(out=rcol(128, 32), in0=elm, scalar1=rcol(96), scalar2=None, op0=ALU.is_equal))
            V(lambda e: e.scalar_tensor_tensor(out=rcol(160, 32), in0=rcol(128, 32), scalar=-BIG, in1=elm, op0=ALU.mult, op1=ALU.add))
            V(lambda e: e.tensor_reduce(out=rcol(97), in_=rcol(160, 32), axis=AX.X, op=ALU.max))
            V(lambda e: e.tensor_scalar(out=rcol(192, 32), in0=rcol(160, 32), scalar1=rcol(97), scalar2=None, op0=ALU.is_equal))
            V(lambda e: e.tensor_tensor(out=rcol(98), in0=rcol(97), in1=rcol(96), op=ALU.subtract))
            P.op("act", lambda e: e.activation(out=rcol(99), in_=rcol(98), func=AF.Exp), r=[k], w=[k])
            V(lambda e: e.tensor_scalar(out=rcol(100), in0=rcol(99), scalar1=1.0, scalar2=None, op0=ALU.add))
            V(lambda e: e.reciprocal(out=rcol(101), in_=rcol(100)))
            V(lambda e: e.tensor_tensor(out=rcol(102), in0=rcol(101), in1=rcol(42), op=ALU.mult))
            V(lambda e: e.tensor_tensor(out=rcol(103), in0=rcol(102), in1=rcol(99), op=ALU.mult))
            V(lambda e: e.tensor_scalar(out=rcol(224, 32), in0=rcol(128, 32), scalar1=rcol(102), scalar2=None, op0=ALU.mult))
            P.op("dve", lambda e: e.scalar_tensor_tensor(out=comb[:, bl, :], in0=rcol(192, 32), scalar=rcol(103), in1=rcol(224, 32), op0=ALU.mult, op1=ALU.add),
                 r=[k], w=["comb"])


        w1v = w1.rearrange("e (c p) n -> e p c n", p=128)
        w3v = w3.rearrange("e (c p) n -> e p c n", p=128)
        w2v = w2.rearrange("e (c p) n -> e p c n", p=128)
        slot13 = [0]
        slot2 = [0]
        loaded13 = {}
        loaded2 = {}

        def load13(e_, hh):
            s_ = slot13[0] % 3
            slot13[0] += 1
            loaded13[(e_, hh)] = s_
            P.dma("pool", w13[s_][:, 0, :, :], w1v[e_, :, :, hh * 256:(hh + 1) * 256], w=[("w13", s_)])
            P.dma("pool", w13[s_][:, 1, :, :], w3v[e_, :, :, hh * 256:(hh + 1) * 256], w=[("w13", s_)])

        def load2(e_, kh):
            s_ = slot2[0] % 3
            slot2[0] += 1
            loaded2[(e_, kh)] = s_
            P.dma("pool", w2b[s_], w2v[e_, :, kh * 2:(kh + 1) * 2, :], w=[("w2", s_)])

        def H_phase(e_):
            hb = hid[e_ % 2]
            hk = ("hid", e_ % 2)
            cnt = 0
            for hh in range(2):
                s_ = loaded13[(e_, hh)]
                for tg in range(2):
                    for mm in range(2):
                        m = 2 * hh + mm
                        b1 = (cnt % 2) * 2
                        b3 = b1 + 1
                        cnt += 1
                        for c in range(16):
                            P.op("pe", lambda e: e.matmul(ps[b1], lhsT=w13[s_][:, 0, c, mm * 128:(mm + 1) * 128], rhs=hnT[:, c, tg * 512:(tg + 1) * 512],
                                                          start=(c == 0), stop=(c == 15)), r=[("w13", s_), "hnT"], w=[("ps", b1)], signal=(c == 15))
                        for c in range(16):
                            P.op("pe", lambda e: e.matmul(ps[b3], lhsT=w13[s_][:, 1, c, mm * 128:(mm + 1) * 128], rhs=hnT[:, c, tg * 512:(tg + 1) * 512],
                                                          start=(c == 0), stop=(c == 15)), r=[("w13", s_), "hnT"], w=[("ps", b3)], signal=(c == 15))
                        sl = sil[cnt % 2]
                        P.op("act", lambda e: e.activation(out=sl, in_=ps[b1], func=AF.Silu), r=[("ps", b1)], w=[("sil", cnt % 2)])
                        P.op("dve", lambda e: e.tensor_tensor(out=hb[:, tg, m, :], in0=sl, in1=ps[b3], op=ALU.mult),
                             r=[("sil", cnt % 2), ("ps", b3)], w=[hk])
                        if e_ == 0:
                            route_block(cnt - 1)

        ycnt = [0]

        def Y_phase(e_):
            hb = hid[e_ % 2]
            hk = ("hid", e_ % 2)
            for bl in range(8):
                tg = bl // 4
                tb_ = bl % 4
                for cg in range(4):
                    by = 4 + ycnt[0] % 3
                    ycnt[0] += 1
                    for m in range(4):
                        s_ = loaded2[(e_, m // 2)]
                        P.op("pe", lambda e: e.matmul(ps[by], lhsT=hb[:, tg, m, tb_ * 128:(tb_ + 1) * 128], rhs=w2b[s_][:, m % 2, cg * 512:(cg + 1) * 512],
                                                      start=(m == 0), stop=(m == 3)), r=[hk, ("w2", s_)], w=[("ps", by)], signal=(m == 3))
                    hsl = h1[:, bl, cg * 512:(cg + 1) * 512]
                    P.op("dve", lambda e: e.scalar_tensor_tensor(out=hsl, in0=ps[by], scalar=comb[:, bl, e_:e_ + 1], in1=hsl, op0=ALU.mult, op1=ALU.add),
                         r=[("ps", by), "comb", ("h1", bl)], w=[("h1", bl)])

        NEX = NE if stage >= 6 else 2
        load13(0, 0)
        load13(0, 1)
        load2(0, 0)
        for e_ in range(NEX):
            if e_ + 1 < NEX:
                load13(e_ + 1, 0)
            load2(e_, 1)
            H_phase(e_)
            if e_ + 1 < NEX:
                load13(e_ + 1, 1)
                load2(e_ + 1, 0)
            Y_phase(e_)

        if stage <= 6:
            for bl in range(8):
                P.dma("sp", out_d[bl * 128:(bl + 1) * 128, :], h1[:, bl, :], r=[("h1", bl)])
            return _finish(nc, P, out_d)
        P.barrier()
        P.release(m_d)
        hi_save = P.mark()
        P.release(m_persist)
        gple = P.alloc("gple_bc", [128, D], F32)
        bple = P.alloc("bple_bc", [128, D], F32)
        wpl = P.alloc("wpl", [128, 2, D], BF16)
        xs0 = P.alloc("xs_f0", [128, D], BF16)
        xs1 = P.alloc("xs_f1", [128, D], BF16)
        assert P.mark() <= m_keep
        P.release(hi_save)
        wg = [P.alloc(f"wg{i}", [128, 16, 512], BF16) for i in range(2)]
        pT = P.alloc("pT", [128, 2, TOK], BF16)
        pblk = [P.alloc(f"pblk{i}", [128, 256], F32) for i in range(2)]
        pbf = [P.alloc(f"pbf{i}", [128, 256], BF16) for i in range(2)]
        pre = [P.alloc(f"pre{i}", [128, 512], F32) for i in range(2)]
        obuf = [P.alloc(f"obuf{i}", [128, 512], F32) for i in range(3)]
        P.dma("sp", gple, prow[0:1, R_NPLE:R_NPLE + D].partition_broadcast(128), w=["gple"])
        P.dma("sp", bple, prow[0:1, R_BPLE:R_BPLE + D].partition_broadcast(128), w=["bple"])
        P.dma("pool", wpl, w_ple.rearrange("(c p) n -> p c n", p=128), w=["wpl"])
        w_pg_v = w_pg.rearrange("(c p) n -> p c n", p=128)
        for bl in range(8):
            rmsnorm_T(h1[:, bl, :], 128, gple, "gple", hnT, bl * 128, ("h1", bl), "hnT", [xs0, xs1][bl % 2], ("xs", bl % 2), banks=[4, 5])
            P.dma("sp", pblk[bl % 2], po[bl * 128:(bl + 1) * 128, :], w=[("pblk", bl % 2)])
            P.op("dve", lambda e: e.tensor_copy(out=pbf[bl % 2], in_=pblk[bl % 2]), r=[("pblk", bl % 2)], w=[("pbf", bl % 2)])
            for c in range(2):
                P.op("pe", lambda e: e.transpose(out=psb[6][:, c * 128:(c + 1) * 128], in_=pbf[bl % 2][:, c * 128:(c + 1) * 128], identity=ident),
                     r=[("pbf", bl % 2), "ident"], w=[("ps", 6)], signal=(c == 1))
            P.op("act", lambda e: e.copy(out=pT[:, :, bl * 128:(bl + 1) * 128], in_=psb[6][:, 0:256].rearrange("p (c t) -> p c t", c=2)),
                 r=[("ps", 6)], w=["pT"])
        oc = 0
        for cg in range(4):
            wgb = wg[cg % 2]
            wgk = ("wg", cg % 2)
            P.dma("pool", wgb[:, 0:8, :], w_pg_v[:, 0:8, cg * 512:(cg + 1) * 512], w=[wgk])
            P.dma("pool", wgb[:, 8:16, :], w_pg_v[:, 8:16, cg * 512:(cg + 1) * 512], w=[wgk])
            for bl in range(8):
                bg = (bl % 2) * 2
                bp = bg + 1
                for c in range(16):
                    P.op("pe", lambda e: e.matmul(ps[bg], lhsT=hnT[:, c, bl * 128:(bl + 1) * 128], rhs=wgb[:, c, :], start=(c == 0), stop=(c == 15)),
                         r=["hnT", wgk], w=[("ps", bg)], signal=(c == 15))
                for c in range(2):
                    P.op("pe", lambda e: e.matmul(ps[bp], lhsT=pT[:, c, bl * 128:(bl + 1) * 128], rhs=wpl[:, c, cg * 512:(cg + 1) * 512], start=(c == 0), stop=(c == 1)),
                         r=["pT", "wpl"], w=[("ps", bp)], signal=(c == 1))
                pr = pre[bl % 2]
                prk = ("pre", bl % 2)
                P.op("dve", lambda e: e.tensor_tensor(out=pr, in0=ps[bg], in1=bple[:, cg * 512:(cg + 1) * 512], op=ALU.add), r=[("ps", bg), "bple"], w=[prk])
                P.op("act", lambda e: e.activation(out=pr, in_=pr, func=AF.Sigmoid), r=[prk], w=[prk])
                P.op("dve", lambda e: e.tensor_tensor(out=pr, in0=pr, in1=ps[bp], op=ALU.mult), r=[prk, ("ps", bp)], w=[prk])
                ob_ = obuf[oc % 3]
                obk = ("obuf", oc % 3)
                oc += 1
                P.op("dve", lambda e: e.tensor_tensor(out=ob_, in0=pr, in1=h1[:, bl, cg * 512:(cg + 1) * 512], op=ALU.add), r=[prk, ("h1", bl)], w=[obk])
                P.dma("sp", out_d[bl * 128:(bl + 1) * 128, cg * 512:(cg + 1) * 512], ob_, r=[obk])
        return _finish(nc, P, out_d)


def _finish(nc, P, out_d):
    P.barrier()
    return nc, P


_CACHE = {}


def _host_inputs(inp):
    x = np.ascontiguousarray(inp["x"], dtype=np.float32)
    p = inp["p"]
    pos = inp["positions"].astype(np.int32)
    f32 = np.float32
    prow = np.concatenate([inp["norm_mix"][0], inp["g_out_attn"][0], inp["norm_moe"][0], inp["norm_ple"][0], inp["b_ple_gate"][0],
                           inp["b_group"][0], inp["b_router"][0]]).astype(f32)[None, :]
    pc = np.zeros((128, NCOL), f32)
    pc[:, C_GKV:C_GKV + 2] = inp["g_kv_lat"][0].reshape(2, 128).T
    pc[:, C_GQL:C_GQL + 4] = inp["g_q_lat"][0].reshape(4, 128).T
    gq = inp["g_q_head"][0]
    gk = inp["g_k_head"][0]
    pc[:, C_GQN] = gq[:128]
    pc[:, C_GKN] = gk[:128]
    sw = (np.arange(64) + 32) % 64
    pc[:64, C_GQR] = gq[128:]
    pc[:64, C_GQRS] = gq[128:][sw]
    pc[:64, C_GKR] = gk[128:]
    pc[:64, C_GKRS] = gk[128:][sw]
    freq = (f32(10000.0) ** (-(np.arange(0, 64, 2, dtype=f32)) / f32(64))).astype(f32)
    pc[:64, C_FREQ] = np.concatenate([freq, freq])
    pc[:32, C_SGN] = -1.0
    pc[32:64, C_SGN] = 1.0
    wc = inp["w_conv"][0]
    for ct in range(8):
        for j in range(3):
            pc[:, C_WCONV + ct * 3 + j] = wc[j, ct * 128:(ct + 1) * 128]
    pc[:, C_GOC:C_GOC + 8] = inp["g_out_conv"][0].reshape(8, 128).T
    pc[:, C_PHASE] = 0.0
    pc[:, C_PHASE + 1] = EPS
    shared = {
        "pcols": pc, "prow": prow,
        "w_in": np.ascontiguousarray(inp["w_in"][0]),
        "w_uq": np.ascontiguousarray(inp["w_uq"][0].reshape(512, H * 192)),
        "w_ukv": np.ascontiguousarray(inp["w_ukv"][0].reshape(256, H * 256)),
        "w_out": np.ascontiguousarray(inp["w_out"][0]),
        "w_r": np.ascontiguousarray(np.concatenate([inp["w_group"][0], inp["w_router"][0]], axis=1)),
        "w1": np.ascontiguousarray(inp["w1"][0]), "w3": np.ascontiguousarray(inp["w3"][0]), "w2": np.ascontiguousarray(inp["w2"][0]),
        "w_ple": np.ascontiguousarray(inp["w_ple"][0]), "w_pg": np.ascontiguousarray(inp["w_ple_gate"][0]),
    }
    in_maps = []
    idxs = []
    for c in range(8):
        b, j = c // 4, c % 4
        blocks = [4 * i + j for i in range(8)]
        tok = np.concatenate([np.arange(bk * 128, (bk + 1) * 128) for bk in blocks])
        halo = np.zeros((HALO, D), f32)
        for i, bk in enumerate(blocks):
            t0 = bk * 128
            if t0 >= 2:
                halo[2 * i:2 * i + 2] = x[b, t0 - 2:t0]
        xo = np.concatenate([x[b, tok], halo], axis=0)
        mask = np.zeros((128, 4, 128), f32)
        kk = np.arange(128)[:, None] // 64
        qq = np.arange(128)[None, :] // 64
        for r in range(4):
            if r < j:
                mask[:, r, :] = 1.0
            elif r == j:
                mask[:, r, :] = (kk <= qq).astype(f32)
        m = dict(shared)
        m.update({"xb": x[b], "xo": np.ascontiguousarray(xo), "posb": pos[b][None, :], "poso": np.ascontiguousarray(pos[b][tok][None, :]),
                  "po": np.ascontiguousarray(p[0, b][tok]).astype(f32), "mask": mask.reshape(128, 512)})
        in_maps.append(m)
        idxs.append((b, tok))
    return in_maps, idxs


def kernel(**inp):
    if "nc" not in _CACHE:
        _CACHE["nc"] = build()[0]
    nc = _CACHE["nc"]
    in_maps, idxs = _host_inputs(inp)
    res = run_bass_kernel_spmd(nc, in_maps, core_ids=list(range(8)))
    out = np.zeros((2, S, D), np.float32)
    for c, (b, tok) in enumerate(idxs):
        out[b, tok] = res.results[c]["out"]
    return out
```

```python
import math
from contextlib import ExitStack

import numpy as np
import concourse.bass as bass
import concourse.mybir as mybir
from concourse.bass_utils import run_bass_kernel_spmd

F32 = mybir.dt.float32
BF16 = mybir.dt.bfloat16
I32 = mybir.dt.int32
AF = mybir.ActivationFunctionType
ALU = mybir.AluOpType
AX = mybir.AxisListType

D = 2048
S = 4096
NB_OWN = 8
TOK = 1024
HALO = 16
H = 8
EPS = 1e-6
NE = 32
DE = 512
TWO_PI = 2.0 * math.pi
SCALE = 192.0 ** -0.5
BIG = 1.0e30

SB_LO = 16512
N_WARM = 16
N_FILL = 2
import os
SB_HI = 229344

R_NMIX, R_GOA, R_NMOE, R_NPLE, R_BPLE, R_BR = 0, 2048, 3072, 5120, 7168, 9216
NROW = 9216 + 36
C_GKV, C_GQL, C_GQN, C_GKN, C_GQR, C_GQRS, C_GKR, C_GKRS, C_FREQ, C_SGN, C_WCONV, C_GOC, C_PHASE = 0, 2, 6, 7, 8, 9, 10, 11, 12, 13, 14, 38, 46
NCOL = 48


class Prog:
    ENG = ("pe", "act", "dve", "pool", "sp")

    def __init__(self, nc, es, nds=40):
        self.nc = nc
        self.e = {"pe": nc.tensor, "act": nc.scalar, "dve": nc.vector, "pool": nc.gpsimd, "sp": nc.sync}
        self.sem = {k: es.enter_context(nc.semaphore("s_" + k)) for k in self.ENG}
        self.cnt = {k: 0 for k in self.ENG}
        self.pending = {k: False for k in self.ENG}
        self.seen = {k: {} for k in self.ENG}
        self.lastw = {}
        self.readers = {}
        self.dsem = [es.enter_context(nc.semaphore(f"d{i}")) for i in range(nds)]
        self.dval = [0] * nds
        self.dnext_q = {k: 0 for k in self.ENG}
        self.nsp = 16
        self.off = SB_LO
        self.nalloc = 0
        self.ninst = 0
        self.bmarks = []
        self.hmarks = []

    def alloc(self, name, shape, dtype):
        nbytes = int(np.prod(shape[1:])) * (4 if dtype in (F32, I32) else 2)
        nbytes = (nbytes + 63) // 64 * 64
        assert self.off + nbytes <= SB_HI, f"SBUF overflow at {name}: {self.off + nbytes - SB_HI}"
        self.nalloc += 1
        t = self.nc.alloc_sbuf_tensor_at(f"{name}_{self.nalloc}", list(shape), dtype, offset=self.off)
        self.off += nbytes
        return t.ap()

    def mark(self):
        return self.off

    def release(self, m):
        self.off = m

    def _wait(self, eng, tok):
        if tok[0] == "e":
            _, e2, idx = tok
            if e2 == eng and eng == "pe":
                return
            key = ("e", e2)
            if self.seen[eng].get(key, 0) >= idx:
                return
            self.seen[eng][key] = idx
            self.e[eng].wait_ge(self.sem[e2], idx)
        else:
            _, s, val = tok
            key = ("d", s)
            if self.seen[eng].get(key, 0) >= val:
                return
            self.seen[eng][key] = val
            self.e[eng].wait_ge(self.dsem[s], val)

    def _deps(self, r, w, eng=None):
        toks = []
        for k in r:
            t = self.lastw.get(k)
            if t is not None:
                toks.append(t)
            if isinstance(k, tuple) and k[0] == "ps" and eng in ("act", "dve"):
                for rk, rt in self.readers.get(k, {}).items():
                    if rk[0] == "e" and rk[1] in ("act", "dve") and rk[1] != eng:
                        toks.append(rt)
        for k in w:
            t = self.lastw.get(k)
            if t is not None:
                toks.append(t)
            toks.extend(self.readers.get(k, {}).values())
        return toks

    def _record(self, tok, r, w):
        for k in w:
            self.lastw[k] = tok
            self.readers[k] = {}
        for k in r:
            d = self.readers.setdefault(k, {})
            if tok[0] == "e":
                d[("e", tok[1])] = tok
            else:
                d[("d", tok[1])] = tok

    def op(self, eng, fn, r=(), w=(), signal=True):
        for t in self._deps(r, w, eng):
            self._wait(eng, t)
        ins = fn(self.e[eng])
        self.ninst += 1
        idx = self.cnt[eng] + 1
        if signal:
            ins.then_inc(self.sem[eng], 1)
            self.cnt[eng] = idx
            self.pending[eng] = False
        else:
            self.pending[eng] = True
        self._record(("e", eng, idx), r, w)

    def dma(self, q, out, in_, r=(), w=()):
        for t in self._deps(r, w):
            self._wait(q, t)
        lo, n = (0, self.nsp) if q != "pool" else (self.nsp, len(self.dsem) - self.nsp)
        s = lo + self.dnext_q[q] % n
        self.dnext_q[q] += 1
        if self.dval[s] > 0:
            self._wait(q, ("d", s, self.dval[s]))
        ins = self.e[q].dma_start(out=out, in_=in_)
        self.ninst += 1
        self.dval[s] += 16
        ins.then_inc(self.dsem[s], 16)
        self._record(("d", s, self.dval[s]), r, w)

    def barrier(self):
        self.bmarks.append(dict(self.cnt))
        for k in self.ENG:
            assert not self.pending[k], k
        for eng in self.ENG:
            for e2 in self.ENG:
                if e2 != eng and self.cnt[e2] > 0:
                    self._wait(eng, ("e", e2, self.cnt[e2]))
            for s in range(len(self.dsem)):
                if self.dval[s] > 0:
                    self._wait(eng, ("d", s, self.dval[s]))
        self.lastw = {}
        self.readers = {}


def build(stage=99, debug=False):
    nc = bass.Bass("TRN2", target_bir_lowering=False)
    dt = nc.dram_tensor
    xb = dt("xb", [S, D], F32, kind="ExternalInput").ap()
    xo = dt("xo", [TOK + HALO, D], F32, kind="ExternalInput").ap()
    posb = dt("posb", [1, S], I32, kind="ExternalInput").ap()
    poso = dt("poso", [1, TOK], I32, kind="ExternalInput").ap()
    po = dt("po", [TOK, 256], F32, kind="ExternalInput").ap()
    maskd = dt("mask", [128, 512], F32, kind="ExternalInput").ap()
    pcols_d = dt("pcols", [128, NCOL], F32, kind="ExternalInput").ap()
    prow = dt("prow", [1, NROW], F32, kind="ExternalInput").ap()
    w_in = dt("w_in", [D, 3904], F32, kind="ExternalInput").ap()
    w_uq = dt("w_uq", [512, H * 192], F32, kind="ExternalInput").ap()
    w_ukv = dt("w_ukv", [256, H * 256], F32, kind="ExternalInput").ap()
    w_out = dt("w_out", [D, D], F32, kind="ExternalInput").ap()
    w_r = dt("w_r", [D, 36], F32, kind="ExternalInput").ap()
    nee = NE if stage > 4 else 1
    w1 = dt("w1", [nee, D, DE], F32, kind="ExternalInput").ap()
    w3 = dt("w3", [nee, D, DE], F32, kind="ExternalInput").ap()
    w2 = dt("w2", [nee, DE, D], F32, kind="ExternalInput").ap()
    w_ple = dt("w_ple", [256, D], F32, kind="ExternalInput").ap()
    w_pg = dt("w_pg", [D, D], F32, kind="ExternalInput").ap()
    out_d = dt("out", [TOK, D], F32, kind="ExternalOutput").ap()
    dbg = {}
    if debug:
        dbg["kT"] = dt("dbg_kT", [192, S], F32, kind="ExternalOutput").ap()
        dbg["qT"] = dt("dbg_qT", [192, TOK], F32, kind="ExternalOutput").ap()
        dbg["v"] = dt("dbg_v", [128, 32 * 129], F32, kind="ExternalOutput").ap()
        dbg["yattn"] = dt("dbg_yattn", [TOK, 1024], F32, kind="ExternalOutput").ap()
        dbg["kvnT"] = dt("dbg_kvnT", [128, 2 * S], F32, kind="ExternalOutput").ap()
        dbg["h1"] = dt("dbg_h1", [TOK, D], F32, kind="ExternalOutput").ap()

    w_in_v = w_in.rearrange("(c p) n -> p c n", p=128)

    with ExitStack() as es:
        P = Prog(nc, es)
        ps = [nc.alloc_psum_tensor(f"ps{i}", [128, 512], F32).ap() for i in range(8)]
        psb = [p_.bitcast(BF16) for p_ in ps]

        ident = P.alloc("ident", [128, 128], BF16)
        ones = P.alloc("ones", [128, 128], BF16)
        onesf = P.alloc("onesf", [128, 1], F32)
        pcols = P.alloc("pcols", [128, NCOL], F32)
        stats = P.alloc("stats", [128, 512], F32)
        junk = P.alloc("junk", [128, D], BF16)
        stat_i = [0]

        P.op("pool", lambda e: e.memset(ident, 1.0), w=["ident"])
        P.op("pool", lambda e: e.affine_select(out=ident, in_=ident, pattern=[[-1, 128]], compare_op=ALU.is_equal,
                                               fill=0.0, base=0, channel_multiplier=1), r=["ident"], w=["ident"])
        P.op("pool", lambda e: e.memset(ones, 1.0), w=["ones"])
        P.op("pool", lambda e: e.memset(onesf, 1.0), w=["onesf"])
        P.op("pool", lambda e: e.memset(stats, 0.0), w=["stats"])
        P.dma("sp", pcols, pcols_d, w=["pcols"])

        def col(c, rows=128):
            return pcols[0:rows, c:c + 1]

        def newstat(n=1):
            i = stat_i[0]
            stat_i[0] += n
            assert stat_i[0] <= 512
            return i

        def rstd_from_psum(pss, rows, n, inv_n, tmp, dst, rkeys, tkey, dkey):
            P.op("act", lambda e: e.activation(out=tmp[0:rows, 0:n], in_=pss, func=AF.Ln, scale=inv_n, bias=col(C_PHASE + 1, rows)),
                 r=rkeys + ["pcols"], w=[tkey])
            P.op("act", lambda e: e.activation(out=dst[0:rows, 0:n], in_=tmp[0:rows, 0:n], func=AF.Exp, scale=-0.5), r=[tkey], w=[dkey])

        def rmsnorm_T(src, rows, gbc, gkey, dstT, col0, skey, dkey, xs, xskey, banks, nfeat=D, evac=("act", "dve")):
            si = newstat(3)
            ssq = stats[0:rows, si:si + 1]
            sq = stats[0:rows, si + 1:si + 2]
            rs = stats[0:rows, si + 2:si + 3]
            P.op("act", lambda e: e.activation(out=junk[0:rows, 0:nfeat], in_=src, func=AF.Square, accum_out=ssq),
                 r=[skey, "stats"], w=[("st", si)])
            P.op("act", lambda e: e.activation(out=sq, in_=ssq, func=AF.Sqrt, scale=1.0 / nfeat, bias=col(C_PHASE + 1, rows)),
                 r=[("st", si), "pcols"], w=[("st", si + 1)])
            P.op("dve", lambda e: e.reciprocal(out=rs, in_=sq), r=[("st", si + 1)], w=[("st", si + 2)])
            P.op("dve", lambda e: e.scalar_tensor_tensor(out=xs[0:rows, 0:nfeat], in0=src, scalar=rs, in1=gbc[0:rows, 0:nfeat],
                                                         op0=ALU.mult, op1=ALU.mult),
                 r=[skey, ("st", si + 2), gkey], w=[xskey])
            nch = nfeat // 128
            for hf in range(nch // 8):
                bk = banks[hf % len(banks)]
                for c8 in range(8):
                    c = hf * 8 + c8
                    P.op("pe", lambda e: e.transpose(out=psb[bk][:, c8 * 128:c8 * 128 + rows], in_=xs[0:rows, c * 128:(c + 1) * 128],
                                                     identity=ident[0:rows, 0:rows]),
                         r=[xskey, "ident"], w=[("ps", bk)], signal=(c8 == 7))
                src_v = psb[bk].rearrange("p (c t) -> p c t", c=8)[:, :, 0:rows]
                dst_v = dstT[:, hf * 8:(hf + 1) * 8, col0:col0 + rows]
                if evac[hf % 2] == "act":
                    P.op("act", lambda e: e.copy(out=dst_v, in_=src_v), r=[("ps", bk)], w=[dkey])
                else:
                    P.op(evac[hf % 2], lambda e: e.tensor_copy(out=dst_v, in_=src_v), r=[("ps", bk)], w=[dkey])

        def rope_tables(pos_dram, n0, n, tab, tkey, tmpf, tmpi, tmpi2, q="sp", t0=0):
            ti = tmpi[0:64, 0:n]
            P.dma(q, ti, pos_dram[0:1, n0:n0 + n].partition_broadcast(64), w=[tkey + "_i"])
            pf = tmpf[0:64, 0, 0:n]
            P.op("dve", lambda e: e.tensor_copy(out=pf, in_=ti), r=[tkey + "_i"], w=[tkey + "_pf"])
            ang = tab[0:64, :, t0:t0 + n]
            P.op("dve", lambda e: e.tensor_scalar(out=tab[0:64, 0, t0:t0 + n], in0=pf, scalar1=col(C_FREQ, 64), scalar2=math.pi / 2,
                                                   op0=ALU.mult, op1=ALU.add), r=[tkey + "_pf", "pcols"], w=[tkey])
            P.op("dve", lambda e: e.tensor_scalar(out=tab[0:64, 1, t0:t0 + n], in0=pf, scalar1=col(C_FREQ, 64), scalar2=None,
                                                   op0=ALU.mult), r=[tkey + "_pf", "pcols", tkey], w=[tkey])
            kf = tmpf[0:64, :, 0:n]
            P.op("dve", lambda e: e.tensor_scalar(out=kf, in0=ang, scalar1=1.0 / TWO_PI, scalar2=None, op0=ALU.mult),
                 r=[tkey], w=[tkey + "_pf"])
            kiv = tmpi2[0:64, :, 0:n]
            P.op("dve", lambda e: e.tensor_copy(out=kiv, in_=kf), r=[tkey + "_pf"], w=[tkey + "_ki"])
            P.op("dve", lambda e: e.tensor_copy(out=kf, in_=kiv), r=[tkey + "_ki"], w=[tkey + "_pf"])
            P.op("dve", lambda e: e.tensor_scalar(out=kf, in0=kf, scalar1=-TWO_PI, scalar2=None, op0=ALU.mult),
                 r=[tkey + "_pf"], w=[tkey + "_pf"])
            P.op("dve", lambda e: e.tensor_tensor(out=ang, in0=ang, in1=kf, op=ALU.add), r=[tkey + "_pf", tkey], w=[tkey])
            P.op("dve", lambda e: e.tensor_scalar(out=kf, in0=ang, scalar1=math.pi, scalar2=-TWO_PI, op0=ALU.is_gt, op1=ALU.mult),
                 r=[tkey], w=[tkey + "_pf"])
            P.op("dve", lambda e: e.tensor_tensor(out=ang, in0=ang, in1=kf, op=ALU.add), r=[tkey, tkey + "_pf"], w=[tkey])
            P.op("dve", lambda e: e.tensor_scalar(out=kf, in0=ang, scalar1=-math.pi, scalar2=TWO_PI, op0=ALU.is_lt, op1=ALU.mult),
                 r=[tkey], w=[tkey + "_pf"])
            P.op("dve", lambda e: e.tensor_tensor(out=ang, in0=ang, in1=kf, op=ALU.add), r=[tkey, tkey + "_pf"], w=[tkey])
            P.op("dve", lambda e: e.tensor_scalar(out=ang, in0=ang, scalar1=3.1415925, scalar2=-3.1415925, op0=ALU.min, op1=ALU.max),
                 r=[tkey], w=[tkey])
            P.op("act", lambda e: e.activation(out=tab[0:64, 0, t0:t0 + n], in_=tab[0:64, 0, t0:t0 + n], func=AF.Sin), r=[tkey], w=[tkey])
            P.op("act", lambda e: e.activation(out=tab[0:64, 1, t0:t0 + n], in_=tab[0:64, 1, t0:t0 + n], func=AF.Sin, scale=col(C_SGN, 64)),
                 r=[tkey, "pcols"], w=[tkey])

        m_persist = P.mark()
        ycgT = P.alloc("ycgT", [128, 8, TOK], BF16)
        rsconv = P.alloc("rsconv", [128, 8], F32)
        yanT = P.alloc("yanT", [128, 8, TOK], BF16)
        m_keep = P.mark()
        kvnT = P.alloc("kvnT", [128, 2, S], BF16)
        ropeK = P.alloc("ropeK", [64, S], BF16)
        sqkr = P.alloc("sqkr", [64, S], BF16)
        qnT = P.alloc("qnT", [128, 4, TOK], BF16)
        tabq = P.alloc("tabq", [64, 2, TOK], F32)
        m_ab = P.mark()

        gbc = P.alloc("gmix_bc", [128, D], F32)
        wkv = P.alloc("wkv", [128, 16, 384], BF16)
        xblk = [P.alloc(f"xblk{i}", [128, D], F32) for i in range(2)]
        xs = [P.alloc(f"xs{i}", [128, D], BF16) for i in range(2)]
        xnT = [P.alloc(f"xnT{i}", [128, 16, 512], BF16) for i in range(2)]
        sqt = [P.alloc(f"sqt{i}", [128, 2, 512], BF16) for i in range(2)]
        rbc = [P.alloc(f"rbc{i}", [128, 512], F32) for i in range(2)]
        rtmp = P.alloc("rtmp", [128, 512], F32)
        tabk = [P.alloc(f"tabk{i}", [64, 2, 512], F32) for i in range(2)]
        tmpf = P.alloc("tmpf", [64, 2, 512], F32)
        tmpi = P.alloc("tmpi", [64, 512], I32)
        tmpi2 = P.alloc("tmpi2", [64, 2, 512], I32)
        t1 = P.alloc("t1", [64, 512], F32)
        t2 = P.alloc("t2", [64, 512], F32)

        P.dma("sp", gbc, prow[0:1, R_NMIX:R_NMIX + D].partition_broadcast(128), w=["gbc"])
        P.dma("pool", wkv[:, :, 0:256], w_in_v[:, :, 512:768], w=["wkv"])
        P.dma("pool", wkv[:, :, 256:320], w_in_v[:, :, 768:832], w=["wkv"])
        P.dma("pool", wkv[:, :, 320:352], w_in_v[:, :, 800:832], w=["wkv"])
        P.dma("pool", wkv[:, :, 352:384], w_in_v[:, :, 768:800], w=["wkv"])

        for hq in range(2):
            rope_tables(poso, hq * 512, 512, tabq, "tabq", tmpf, tmpi, tmpi2, t0=hq * 512)
        NG = S // 512
        def emit_blocks(g):
            xg_ = xnT[g % 2]
            for bl in range(4):
                t = g * 4 + bl
                xb_t = xblk[t % 2]
                P.dma("sp", xb_t, xb[t * 128:(t + 1) * 128, :], w=[("xblk", t % 2)])
                rmsnorm_T(xb_t, 128, gbc, "gbc", xg_, bl * 128, ("xblk", t % 2), ("xnT", g % 2), xs[t % 2], ("xs", t % 2), banks=[0, 1])
            rope_tables(posb, g * 512, 512, tabk[g % 2], f"tabk{g % 2}", tmpf, tmpi, tmpi2)

        for g in range(NG):
            emit_blocks(g)
            xg = xnT[g % 2]
            xgk = ("xnT", g % 2)
            tb = tabk[g % 2]
            tbk = f"tabk{g % 2}"
            tiles = [(2, 0, 128), (3, 128, 128), (4, 256, 64), (5, 320, 64)]
            for (bk, c0, m) in tiles:
                for c in range(16):
                    P.op("pe", lambda e: e.matmul(ps[bk][0:m, :], lhsT=wkv[:, c, c0:c0 + m], rhs=xg[:, c, :], start=(c == 0), stop=(c == 15)),
                         r=["wkv", xgk], w=[("ps", bk)], signal=(c == 15))
            sq = sqt[g % 2]
            sqk = ("sqt", g % 2)
            for j in range(2):
                P.op("act", lambda e: e.activation(out=sq[:, j, :], in_=ps[2 + j], func=AF.Square), r=[("ps", 2 + j)], w=[sqk])
            for j in range(2):
                P.op("pe", lambda e: e.matmul(ps[6], lhsT=ones, rhs=sq[:, j, :], start=(j == 0), stop=(j == 1)),
                     r=["ones", sqk], w=[("ps", 6)], signal=(j == 1))
            rb = rbc[g % 2]
            rbk = ("rbc", g % 2)
            rstd_from_psum(ps[6], 128, 512, 1.0 / 256, rtmp, rb, [("ps", 6)], "rtmp", rbk)
            for j in range(2):
                P.op("dve", lambda e: e.scalar_tensor_tensor(out=kvnT[:, j, g * 512:(g + 1) * 512], in0=ps[2 + j], scalar=col(C_GKV + j),
                                                             in1=rb, op0=ALU.mult, op1=ALU.mult),
                     r=[("ps", 2 + j), rbk, "pcols"], w=["kvnT"])
            P.op("act", lambda e: e.activation(out=sqkr[:, g * 512:(g + 1) * 512], in_=ps[4][0:64, :], func=AF.Square),
                 r=[("ps", 4)], w=["sqkr"])
            P.op("dve", lambda e: e.scalar_tensor_tensor(out=t1, in0=ps[4][0:64, :], scalar=col(C_GKR, 64), in1=tb[0:64, 0, :],
                                                         op0=ALU.mult, op1=ALU.mult), r=[("ps", 4), tbk, "pcols"], w=["t1"])
            P.op("dve", lambda e: e.scalar_tensor_tensor(out=t2, in0=ps[5][0:64, :], scalar=col(C_GKRS, 64), in1=tb[0:64, 1, :],
                                                         op0=ALU.mult, op1=ALU.mult), r=[("ps", 5), tbk, "pcols"], w=["t2"])
            P.op("dve", lambda e: e.tensor_tensor(out=ropeK[:, g * 512:(g + 1) * 512], in0=t1, in1=t2, op=ALU.add),
                 r=["t1", "t2"], w=["ropeK"])

        if stage <= 1:
            return _finish(nc, P, out_d)

        P.barrier()
        P.release(m_ab)
        xnTo = P.alloc("xnTo", [128, 16, TOK + HALO], BF16)
        wq = P.alloc("wq", [128, 16, 512], BF16)
        wcv = [P.alloc(f"wcv{i}", [128, 3, 16, 128], BF16) for i in range(2)]
        P.dma("pool", wq, w_in_v[:, :, 0:512], w=["wq"])
        for gi, base in enumerate((832, 1856, 2880)):
            P.dma("pool", wcv[0][:, gi, :, :], w_in_v[:, :, base:base + 128], w=[("wcv", 0)])
        m_b1 = P.mark()
        gbc = P.alloc("gmix_bc", [128, D], F32)
        xblk = [P.alloc(f"xblk{i}", [128, D], F32) for i in range(2)]
        xs = [P.alloc(f"xs{i}", [128, D], BF16) for i in range(2)]
        P.dma("sp", gbc, prow[0:1, R_NMIX:R_NMIX + D].partition_broadcast(128), w=["gbc"])
        for bl in range(NB_OWN + 1):
            rows = 128 if bl < NB_OWN else HALO
            xb_t = xblk[bl % 2]
            P.dma("sp", xb_t[0:rows, :], xo[bl * 128:bl * 128 + rows, :], w=[("xblk", bl % 2)])
            rmsnorm_T(xb_t[0:rows, :], rows, gbc, "gbc", xnTo, bl * 128, ("xblk", bl % 2), "xnTo", xs[bl % 2], ("xs", bl % 2), banks=[6, 7])
        P.barrier()
        P.release(m_b1)
        sq4 = P.alloc("sq4", [128, 4, 512], BF16)
        rb = P.alloc("rbq", [128, 512], F32)
        rtmp = P.alloc("rtmpq", [128, 512], F32)
        cs = [P.alloc(f"cs{i}", [128, 512], F32) for i in range(2)]
        uu = [P.alloc(f"uu{i}", [128, 8, 130], F32) for i in range(2)]
        acc = [P.alloc(f"acc{i}", [128, 512], F32) for i in range(2)]
        yc = [P.alloc(f"yc{i}", [128, 512], F32) for i in range(2)]
        sqsum = P.alloc("sqsum", [128, TOK], F32)
        sqc = P.alloc("sqc", [128, 512], F32)
        hal = P.alloc("hal", [128, 32], F32)

        P.op("pool", lambda e: e.memset(sqsum, 0.0), w=["sqsum"])
        for tg in range(2):
            for mt in range(4):
                for c in range(16):
                    P.op("pe", lambda e: e.matmul(ps[mt], lhsT=wq[:, c, mt * 128:(mt + 1) * 128], rhs=xnTo[:, c, tg * 512:(tg + 1) * 512],
                                                  start=(c == 0), stop=(c == 15)), r=["wq", "xnTo"], w=[("ps", mt)], signal=(c == 15))
                P.op("act", lambda e: e.activation(out=sq4[:, mt, :], in_=ps[mt], func=AF.Square), r=[("ps", mt)], w=["sq4"])
            for mt in range(4):
                P.op("pe", lambda e: e.matmul(ps[4], lhsT=ones, rhs=sq4[:, mt, :], start=(mt == 0), stop=(mt == 3)),
                     r=["ones", "sq4"], w=[("ps", 4)], signal=(mt == 3))
            rstd_from_psum(ps[4], 128, 512, 1.0 / 512, rtmp, rb, [("ps", 4)], "rtmpq", "rbq")
            for mt in range(4):
                P.op("dve", lambda e: e.scalar_tensor_tensor(out=qnT[:, mt, tg * 512:(tg + 1) * 512], in0=ps[mt], scalar=col(C_GQL + mt),
                                                             in1=rb, op0=ALU.mult, op1=ALU.mult),
                     r=[("ps", mt), "rbq", "pcols"], w=["qnT"])
        for ct in range(8):
            wv = wcv[ct % 2]
            wk = ("wcv", ct % 2)
            for gi, base in enumerate((832, 1856, 2880)):
                if ct > 0:
                    P.dma("pool", wv[:, gi, :, :], w_in_v[:, :, base + ct * 128: base + (ct + 1) * 128], w=[wk])
            for gi, c0 in ((1, 0), (2, 16)):
                for c in range(16):
                    P.op("pe", lambda e: e.matmul(ps[6][:, c0:c0 + 16], lhsT=wv[:, gi, c, :], rhs=xnTo[:, c, TOK:TOK + HALO],
                                                  start=(c == 0), stop=(c == 15)), r=[wk, "xnTo"], w=[("ps", 6)], signal=(c == 15))
            P.op("act", lambda e: e.copy(out=hal, in_=ps[6][:, 0:32]), r=[("ps", 6)], w=["hal"])
            for seg in range(2):
                par = (ct * 2 + seg) % 2
                bks = (0, 1, 2) if par == 0 else (3, 4, 5)
                u = uu[par]
                uk = ("uu", par)
                P.op("dve", lambda e: e.tensor_tensor(out=u[:, seg * 4:(seg + 1) * 4, 0:2],
                                                       in0=hal[:, 0:16].rearrange("p (b t) -> p b t", t=2)[:, seg * 4:(seg + 1) * 4, :],
                                                       in1=hal[:, 16:32].rearrange("p (b t) -> p b t", t=2)[:, seg * 4:(seg + 1) * 4, :],
                                                       op=ALU.mult), r=["hal"], w=[uk])
                for gi, bk in ((1, bks[0]), (2, bks[1]), (0, bks[2])):
                    for c in range(16):
                        P.op("pe", lambda e: e.matmul(ps[bk], lhsT=wv[:, gi, c, :], rhs=xnTo[:, c, seg * 512:(seg + 1) * 512],
                                                      start=(c == 0), stop=(c == 15)), r=[wk, "xnTo"], w=[("ps", bk)], signal=(c == 15))
                P.op("act", lambda e: e.copy(out=cs[par], in_=ps[bks[0]]), r=[("ps", bks[0])], w=[("cs", par)])
                P.op("dve", lambda e: e.tensor_tensor(out=u[:, seg * 4:(seg + 1) * 4, 2:130], in0=cs[par].rearrange("p (b t) -> p b t", t=128),
                                                      in1=ps[bks[1]].rearrange("p (b t) -> p b t", t=128), op=ALU.mult),
                     r=[("cs", par), ("ps", bks[1])], w=[uk])
                a = acc[par].rearrange("p (b t) -> p b t", t=128)
                ak = ("acc", par)
                us = u[:, seg * 4:(seg + 1) * 4, :]
                P.op("act", lambda e: e.activation(out=a, in_=us[:, :, 2:130], func=AF.Copy, scale=col(C_WCONV + ct * 3 + 2)),
                     r=[uk, "pcols"], w=[ak])
                P.op("dve", lambda e: e.scalar_tensor_tensor(out=a, in0=us[:, :, 1:129], scalar=col(C_WCONV + ct * 3 + 1), in1=a,
                                                             op0=ALU.mult, op1=ALU.add), r=[uk, "pcols", ak], w=[ak])
                P.op("dve", lambda e: e.scalar_tensor_tensor(out=a, in0=us[:, :, 0:128], scalar=col(C_WCONV + ct * 3 + 0), in1=a,
                                                             op0=ALU.mult, op1=ALU.add), r=[uk, "pcols", ak], w=[ak])
                P.op("dve", lambda e: e.tensor_tensor(out=yc[par], in0=acc[par], in1=ps[bks[2]], op=ALU.mult),
                     r=[ak, ("ps", bks[2])], w=[("yc", par)])
                P.op("act", lambda e: e.activation(out=ycgT[:, ct, seg * 512:(seg + 1) * 512], in_=yc[par], func=AF.Copy, scale=col(C_GOC + ct)),
                     r=[("yc", par), "pcols"], w=["ycgT"])
                P.op("act", lambda e: e.activation(out=sqc, in_=yc[par], func=AF.Square), r=[("yc", par)], w=["sqc"])
                P.op("dve", lambda e: e.tensor_tensor(out=sqsum[:, seg * 512:(seg + 1) * 512], in0=sqsum[:, seg * 512:(seg + 1) * 512],
                                                       in1=sqc, op=ALU.add), r=["sqc", "sqsum"], w=["sqsum"])
        for bl in range(8):
            P.op("pe", lambda e: e.matmul(ps[7][:, bl:bl + 1], lhsT=sqsum[:, bl * 128:(bl + 1) * 128], rhs=onesf, start=True, stop=True),
                 r=["sqsum", "onesf"], w=[("ps", 7)], signal=(bl == 7))
        si = newstat(8)
        rstd_from_psum(ps[7][:, 0:8], 128, 8, 1.0 / 1024, stats[:, si:si + 8], rsconv, [("ps", 7)], ("st", si), "rsconv")

        if stage <= 2:
            return _finish(nc, P, out_d)
        P.barrier()
        P.release(m_ab)
        yattn = P.alloc("yattn", [128, 8, 1024], F32)
        goa = P.alloc("goa_bc", [128, 1024], F32)
        xsd = [P.alloc(f"xsd{i}", [128, D], BF16) for i in range(2)]
        P.dma("sp", goa, prow[0:1, R_GOA:R_GOA + 1024].partition_broadcast(128), w=["goa"])
        wukv = P.alloc("wukv", [128, 2, H * 256], BF16)
        wuq = P.alloc("wuq", [128, 4, H * 256], BF16)
        KnT = P.alloc("KnT", [128, S], BF16)
        KrT = P.alloc("KrT", [64, S], BF16)
        V1 = P.alloc("V1", [128, 32, 129], BF16)
        QnT = P.alloc("QnT", [128, TOK], BF16)
        QrT = P.alloc("QrT", [64, TOK], BF16)
        PT = [P.alloc(f"PT{i}", [128, TOK], BF16) for i in range(3)]
        mask = P.alloc("mask", [128, 512], BF16)
        sqk = [P.alloc(f"sqk{i}", [128, 512], BF16) for i in range(2)]
        sqr = [P.alloc(f"sqr{i}", [64, 512], BF16) for i in range(2)]
        rb2 = [P.alloc(f"rb2{i}", [128, 512], F32) for i in range(2)]
        rt2 = [P.alloc(f"rt2{i}", [128, 512], F32) for i in range(2)]
        q1 = P.alloc("q1", [64, 512], F32)
        q2 = P.alloc("q2", [64, 512], F32)
        rinv = P.alloc("rinv", [128, 8], F32)

        P.dma("pool", mask, maskd, w=["mask"])
        w_ukv_v = w_ukv.rearrange("(c p) n -> p c n", p=128)
        wuq_v = w_uq.rearrange("(c p) (h d) -> p c h d", p=128, h=H)
        wuq_h = wuq.rearrange("p c (h d) -> p c h d", h=H)
        P.op("pool", lambda e: e.memset(V1[:, :, 128:129], 1.0), w=["V1"])

        _save_c = P.mark()
        P.release(m_keep)
        h1 = P.alloc("h1", [128, 8, D], F32)
        hnT = P.alloc("hnT", [128, 16, TOK], BF16)
        m_d = P.mark()
        wo = [P.alloc(f"wo{i}", [128, 16, 512], BF16) for i in range(2)]
        m_wo_end = P.mark()
        P.release(_save_c)
        w_out_v = w_out.rearrange("(c p) n -> p c n", p=128)
        for h in range(H):
            P.hmarks.append(('build', h, dict(P.cnt)))
            if os.environ.get('K_F1', '1') == '1':
                P.dma("pool", wukv[:, :, h * 256:(h + 1) * 256], w_ukv_v[:, :, h * 256:(h + 1) * 256], w=["wukv"])
                for c4 in range(4):
                    P.dma("pool", wuq_h[:, c4, h, 0:192], wuq_v[:, c4, h, :], w=["wuq"])
                    P.dma("pool", wuq_h[:, c4, h, 192:224], wuq_v[:, c4, h, 160:192], w=["wuq"])
                    P.dma("pool", wuq_h[:, c4, h, 224:256], wuq_v[:, c4, h, 128:160], w=["wuq"])
            elif h == 0:
                P.dma("pool", wukv, w_ukv_v, w=["wukv"])
                for c4 in range(4):
                    P.dma("pool", wuq_h[:, c4, :, 0:192], wuq_v[:, c4, :, :], w=["wuq"])
                    P.dma("pool", wuq_h[:, c4, :, 192:224], wuq_v[:, c4, :, 160:192], w=["wuq"])
                    P.dma("pool", wuq_h[:, c4, :, 224:256], wuq_v[:, c4, :, 128:160], w=["wuq"])
            for g in range(NG):
                bk = g % 2
                bs = 2 + g % 2
                for j in range(2):
                    P.op("pe", lambda e: e.matmul(ps[bk], lhsT=wukv[:, j, h * 256:h * 256 + 128], rhs=kvnT[:, j, g * 512:(g + 1) * 512],
                                                  start=(j == 0), stop=(j == 1)), r=["wukv", "kvnT"], w=[("ps", bk)], signal=(j == 1))
                sk = sqk[g % 2]
                P.op("act", lambda e: e.activation(out=sk, in_=ps[bk], func=AF.Square), r=[("ps", bk)], w=[("sqk", g % 2)])
                P.op("pe", lambda e: e.matmul(ps[bs], lhsT=ones, rhs=sk, start=True, stop=False), r=["ones", ("sqk", g % 2)], w=[("ps", bs)], signal=False)
                P.op("pe", lambda e: e.matmul(ps[bs], lhsT=ones[0:64, :], rhs=sqkr[:, g * 512:(g + 1) * 512], start=False, stop=True),
                     r=["ones", "sqkr"], w=[("ps", bs)])
                rstd_from_psum(ps[bs], 128, 512, 1.0 / 192, rt2[g % 2], rb2[g % 2], [("ps", bs)], ("rt2", g % 2), ("rb2", g % 2))
                P.op("dve", lambda e: e.scalar_tensor_tensor(out=KnT[:, g * 512:(g + 1) * 512], in0=ps[bk], scalar=col(C_GKN), in1=rb2[g % 2],
                                                             op0=ALU.mult, op1=ALU.mult), r=[("ps", bk), ("rb2", g % 2), "pcols"], w=["KnT"])
                P.op("dve", lambda e: e.tensor_tensor(out=KrT[:, g * 512:(g + 1) * 512], in0=ropeK[:, g * 512:(g + 1) * 512],
                                                       in1=rb2[g % 2][0:64, :], op=ALU.mult), r=["ropeK", ("rb2", g % 2)], w=["KrT"])
                vb = 4
                for kb4 in range(4):
                    kb = g * 4 + kb4
                    for j in range(2):
                        P.op("pe", lambda e: e.matmul(ps[vb][:, kb4 * 128:(kb4 + 1) * 128], lhsT=kvnT[:, j, kb * 128:(kb + 1) * 128],
                                                      rhs=wukv[:, j, h * 256 + 128:h * 256 + 256], start=(j == 0), stop=(j == 1)),
                             r=["wukv", "kvnT"], w=[("ps", vb)], signal=(j == 1 and kb4 == 3))
                P.op("act", lambda e: e.copy(out=V1[:, g * 4:(g + 1) * 4, 0:128], in_=ps[vb].rearrange("p (b d) -> p b d", b=4)),
                     r=[("ps", vb)], w=["V1"])
            for tg in range(2):
                tsl = slice(tg * 512, (tg + 1) * 512)
                bq = 0
                for j in range(4):
                    P.op("pe", lambda e: e.matmul(ps[bq], lhsT=wuq[:, j, h * 256:h * 256 + 128], rhs=qnT[:, j, tsl], start=(j == 0), stop=(j == 3)),
                         r=["wuq", "qnT"], w=[("ps", bq)], signal=(j == 3))
                for j in range(4):
                    P.op("pe", lambda e: e.matmul(ps[1][0:64, :], lhsT=wuq[:, j, h * 256 + 128:h * 256 + 192], rhs=qnT[:, j, tsl], start=(j == 0), stop=(j == 3)),
                         r=["wuq", "qnT"], w=[("ps", 1)], signal=(j == 3))
                for j in range(4):
                    P.op("pe", lambda e: e.matmul(ps[4][0:64, :], lhsT=wuq[:, j, h * 256 + 192:h * 256 + 256], rhs=qnT[:, j, tsl], start=(j == 0), stop=(j == 3)),
                         r=["wuq", "qnT"], w=[("ps", 4)], signal=(j == 3))
                P.op("act", lambda e: e.activation(out=sqk[tg], in_=ps[bq], func=AF.Square), r=[("ps", bq)], w=[("sqk", tg)])
                P.op("act", lambda e: e.activation(out=sqr[tg], in_=ps[1][0:64, :], func=AF.Square), r=[("ps", 1)], w=[("sqr", tg)])
                bs = 2 + tg
                P.op("pe", lambda e: e.matmul(ps[bs], lhsT=ones, rhs=sqk[tg], start=True, stop=False), r=["ones", ("sqk", tg)], w=[("ps", bs)], signal=False)
                P.op("pe", lambda e: e.matmul(ps[bs], lhsT=ones[0:64, :], rhs=sqr[tg], start=False, stop=True), r=["ones", ("sqr", tg)], w=[("ps", bs)])
                rstd_from_psum(ps[bs], 128, 512, 1.0 / 192, rt2[tg], rb2[tg], [("ps", bs)], ("rt2", tg), ("rb2", tg))
                P.op("dve", lambda e: e.scalar_tensor_tensor(out=QnT[:, tsl], in0=ps[bq], scalar=col(C_GQN), in1=rb2[tg], op0=ALU.mult, op1=ALU.mult),
                     r=[("ps", bq), ("rb2", tg), "pcols"], w=["QnT"])
                P.op("dve", lambda e: e.scalar_tensor_tensor(out=q1, in0=ps[1][0:64, :], scalar=col(C_GQR, 64), in1=tabq[0:64, 0, tsl],
                                                             op0=ALU.mult, op1=ALU.mult), r=[("ps", 1), "tabq", "pcols"], w=["q1"])
                P.op("dve", lambda e: e.scalar_tensor_tensor(out=q2, in0=ps[4][0:64, :], scalar=col(C_GQRS, 64), in1=tabq[0:64, 1, tsl],
                                                             op0=ALU.mult, op1=ALU.mult), r=[("ps", 4), "tabq", "pcols"], w=["q2"])
                P.op("dve", lambda e: e.tensor_tensor(out=q1, in0=q1, in1=q2, op=ALU.add), r=["q1", "q2"], w=["q1"])
                P.op("dve", lambda e: e.tensor_tensor(out=QrT[:, tsl], in0=q1, in1=rb2[tg][0:64, :], op=ALU.mult), r=["q1", ("rb2", tg)], w=["QrT"])
            P.hmarks.append(('attn', h, dict(P.cnt)))
            if h == H - 1:
                dead = ["kvnT", "ropeK", "sqkr", "qnT", "tabq"]
                for bl in range(6):
                    P.dma("sp", h1[:, bl, :], xo[bl * 128:(bl + 1) * 128, :], w=[("h1", bl)] + dead)
                P.dma("pool", wo[0][:, 0:8, :], w_out_v[:, 0:8, 0:512], w=[("wo", 0), "wukv", "wuq"])
                P.dma("pool", wo[0][:, 8:16, :], w_out_v[:, 8:16, 0:512], w=[("wo", 0), "wukv", "wuq"])
            for bk in (5, 6, 7):
                P.op("dve", lambda e: e.memset(ps[bk], 0.0), w=[("ps", bk)])
            sbank = [0]

            def emit_S(kb):
                i0_ = kb // 4
                q0_ = i0_ * 128
                chunks = [(q0_, min(q0_ + 512, TOK))]
                if chunks[0][1] < TOK:
                    chunks.append((chunks[0][1], TOK))
                res_ = []
                for (a0, a1) in chunks:
                    sb_ = sbank[0] % 4
                    sbank[0] += 1
                    n = a1 - a0
                    P.op("pe", lambda e: e.matmul(ps[sb_][:, 0:n], lhsT=KnT[:, kb * 128:(kb + 1) * 128], rhs=QnT[:, a0:a1], start=True, stop=False),
                         r=["KnT", "QnT"], w=[("ps", sb_)], signal=False)
                    P.op("pe", lambda e: e.matmul(ps[sb_][:, 0:n], lhsT=KrT[:, kb * 128:(kb + 1) * 128], rhs=QrT[:, a0:a1], start=False, stop=True),
                         r=["KrT", "QrT"], w=[("ps", sb_)])
                    res_.append((a0, a1, sb_))
                return res_

            for wi in range(N_WARM):
                P.op("pe", lambda e: e.matmul(ps[4], lhsT=ones, rhs=KnT[:, (wi % 8) * 512:(wi % 8 + 1) * 512], start=True, stop=True),
                     r=["ones", "KnT"], w=[("ps", 4)], signal=(wi == N_WARM - 1))
            cur = emit_S(0)
            for kb in range(32):
                i0 = kb // 4
                q0 = i0 * 128
                pt = PT[kb % 3]
                ptk = ("PT", kb % 3)
                for (a0, a1, sb_) in cur:
                    P.op("act", lambda e: e.activation(out=pt[:, a0:a1], in_=ps[sb_][:, 0:a1 - a0], func=AF.Exp, scale=SCALE), r=[("ps", sb_)], w=[ptk])
                P.op("dve", lambda e: e.tensor_tensor(out=pt[:, q0:q0 + 128], in0=pt[:, q0:q0 + 128], in1=mask[:, (kb % 4) * 128:(kb % 4 + 1) * 128],
                                                       op=ALU.mult), r=[ptk, "mask"], w=[ptk])
                if kb + 1 < 32:
                    cur = emit_S(kb + 1)
                nfill = N_FILL if kb < 16 else max(N_FILL - 1, 0)
                for wi in range(nfill):
                    P.op("pe", lambda e: e.matmul(ps[4], lhsT=ones, rhs=KnT[:, (wi % 8) * 512:(wi % 8 + 1) * 512], start=True, stop=True),
                         r=["ones", "KnT"], w=[("ps", 4)], signal=(wi == nfill - 1))
                for i in range(i0, 8):
                    ob = 5 + i // 3
                    oc = (i % 3) * 129
                    P.op("pe", lambda e: e.matmul(ps[ob][:, oc:oc + 129], lhsT=pt[:, i * 128:(i + 1) * 128], rhs=V1[:, kb, :], start=False, stop=False,
                                                  skip_group_check=True), r=[ptk, "V1", ("ps", ob)], w=[("pso", i)], signal=(i == 7))
                if kb % 4 == 3:
                    i = i0
                    ob = 5 + i // 3
                    oc = (i % 3) * 129
                    P.op("dve", lambda e: e.reciprocal(out=rinv[:, i:i + 1], in_=ps[ob][:, oc + 128:oc + 129]), r=[("pso", i), ("ps", ob)], w=[("rinv", i)])
                    P.op("act", lambda e: e.activation(out=yattn[:, i, h * 128:(h + 1) * 128], in_=ps[ob][:, oc:oc + 128], func=AF.Copy, scale=rinv[:, i:i + 1]),
                         r=[("pso", i), ("ps", ob), ("rinv", i)], w=[("yattn", i)])
        if debug:
            P.barrier()
            for i in range(8):
                P.dma("sp", dbg["yattn"][i * 128:(i + 1) * 128, :], yattn[:, i, :], r=[("yattn", i)])
        if stage <= 3:
            return _finish(nc, P, out_d)
        for i in range(8):
            rmsnorm_T(yattn[:, i, :], 128, goa, "goa", yanT, i * 128, ("yattn", i), "yanT", xsd[i % 2], ("xs", i % 2), banks=[0, 1], nfeat=1024)
        P.barrier()
        P.release(m_keep)
        P.off = m_wo_end
        gmoe = P.alloc("gmoe_bc", [128, D], F32)
        xs0 = P.alloc("xs_d0", [128, D], BF16)
        xs1 = P.alloc("xs_d1", [128, D], BF16)
        P.dma("sp", gmoe, prow[0:1, R_NMOE:R_NMOE + D].partition_broadcast(128), w=["gmoe"])
        for bl in range(6, 8):
            P.dma("sp", h1[:, bl, :], xo[bl * 128:(bl + 1) * 128, :], w=[("h1", bl)])
        for cg in range(4):
            wob = wo[cg % 2]
            wok = ("wo", cg % 2)
            if cg > 0:
                P.dma("pool", wob[:, 0:8, :], w_out_v[:, 0:8, cg * 512:(cg + 1) * 512], w=[wok])
                P.dma("pool", wob[:, 8:16, :], w_out_v[:, 8:16, cg * 512:(cg + 1) * 512], w=[wok])
            for bl in range(8):
                ba = (bl % 2) * 2
                bc = ba + 1
                for c in range(8):
                    P.op("pe", lambda e: e.matmul(ps[ba], lhsT=yanT[:, c, bl * 128:(bl + 1) * 128], rhs=wob[:, c, :], start=(c == 0), stop=(c == 7)),
                         r=["yanT", wok], w=[("ps", ba)], signal=(c == 7))
                for c in range(8):
                    P.op("pe", lambda e: e.matmul(ps[bc], lhsT=ycgT[:, c, bl * 128:(bl + 1) * 128], rhs=wob[:, 8 + c, :], start=(c == 0), stop=(c == 7)),
                         r=["ycgT", wok], w=[("ps", bc)], signal=(c == 7))
                hsl = h1[:, bl, cg * 512:(cg + 1) * 512]
                P.op("dve", lambda e: e.tensor_tensor(out=hsl, in0=ps[ba], in1=hsl, op=ALU.add), r=[("ps", ba), ("h1", bl)], w=[("h1", bl)])
                P.op("dve", lambda e: e.scalar_tensor_tensor(out=hsl, in0=ps[bc], scalar=rsconv[:, bl:bl + 1], in1=hsl, op0=ALU.mult, op1=ALU.add),
                     r=[("ps", bc), ("h1", bl), "rsconv"], w=[("h1", bl)])
        if debug:
            P.barrier()
            for bl in range(8):
                P.dma("sp", dbg["h1"][bl * 128:(bl + 1) * 128, :], h1[:, bl, :], r=[("h1", bl)])
        for bl in range(8):
            rmsnorm_T(h1[:, bl, :], 128, gmoe, "gmoe", hnT, bl * 128, ("h1", bl), "hnT", [xs0, xs1][bl % 2], ("xs", bl % 2), banks=[4, 5])
        if stage <= 4:
            for bl in range(8):
                P.dma("sp", out_d[bl * 128:(bl + 1) * 128, :], h1[:, bl, :], r=[("h1", bl)])
            return _finish(nc, P, out_d)
        P.barrier()
        P.release(m_d)
        wr = P.alloc("wr", [128, 16, 36], BF16)
        brb = P.alloc("brb", [128, 36], F32)
        comb = P.alloc("comb", [128, 8, 32], F32)
        rt = P.alloc("rt", [128, 256], F32)
        w13 = [P.alloc(f"w13_{i}", [128, 2, 16, 256], BF16) for i in range(3)]
        hid = [P.alloc(f"hid{i}", [128, 2, 4, 512], BF16) for i in range(2)]
        hi_save = P.mark()
        P.release(m_persist)
        w2b = [P.alloc(f"w2_{i}", [128, 2, D], BF16) for i in range(3)]
        sil = [P.alloc(f"sil{i}", [128, 512], F32) for i in range(2)]
        assert P.mark() <= m_keep
        P.release(hi_save)
        P.dma("pool", wr, w_r.rearrange("(c p) n -> p c n", p=128), w=["wr"])
        P.dma("sp", brb, prow[0:1, R_BR:R_BR + 36].partition_broadcast(128), w=["brb"])

        def rcol(i, n=1):
            return rt[:, i:i + n]

        for bl in range(8):
            for c in range(16):
                P.op("pe", lambda e: e.matmul(ps[7][:, bl * 36:(bl + 1) * 36], lhsT=hnT[:, c, bl * 128:(bl + 1) * 128], rhs=wr[:, c, :], start=(c == 0), stop=(c == 15)),
                     r=["hnT", "wr"], w=[("psr", bl)], signal=(c == 15))

        def route_block(bl):
            lg = rcol(0, 36)
            k = "rt"
            V = lambda f: P.op("dve", f, r=[k, "brb", ("psr", bl)], w=[k])
            P.op("dve", lambda e: e.tensor_tensor(out=lg, in0=ps[7][:, bl * 36:(bl + 1) * 36], in1=brb, op=ALU.add), r=[("psr", bl), "brb"], w=[k])
            gl = rcol(0, 4)
            el = rcol(4, 32)
            V(lambda e: e.tensor_reduce(out=rcol(40), in_=gl, axis=AX.X, op=ALU.max))
            V(lambda e: e.tensor_scalar(out=rcol(44, 4), in0=gl, scalar1=rcol(40), scalar2=None, op0=ALU.subtract))
            P.op("act", lambda e: e.activation(out=rcol(48, 4), in_=rcol(44, 4), func=AF.Exp, accum_out=rcol(41)), r=[k], w=[k])
            V(lambda e: e.reciprocal(out=rcol(42), in_=rcol(41)))
            V(lambda e: e.tensor_scalar(out=rcol(52, 4), in0=gl, scalar1=rcol(40), scalar2=None, op0=ALU.is_equal))
            V(lambda e: e.tensor_scalar(out=rcol(56, 4), in0=rcol(52, 4), scalar1=1.0, scalar2=BIG, op0=ALU.subtract, op1=ALU.mult))
            elm = rcol(64, 32)
            V(lambda e: e.tensor_tensor(out=elm.rearrange("p (g x) -> p g x", g=4), in0=el.rearrange("p (g x) -> p g x", g=4),
                                        in1=rcol(56, 4).unsqueeze(2).to_broadcast([128, 4, 8]), op=ALU.add))
            V(lambda e: e.tensor_reduce(out=rcol(96), in_=elm, axis=AX.X, op=ALU.max))
            V(lambda e: e.tensor_scalar(out=rcol(128, 32), in0=elm, scalar1=rcol(96), scalar2=None, op0=ALU.is_equal))
            V(lambda e: e.scalar_tensor_tensor(out=rcol(160, 32), in0=rcol(128, 32), scalar=-BIG, in1=elm, op0=ALU.mult, op1=ALU.add))
            V(lambda e: e.tensor_reduce(out=rcol(97), in_=rcol(160, 32), axis=AX.X, op=ALU.max))
            V(lambda e: e.tensor_scalar(out=rcol(192, 32), in0=rcol(160, 32), scalar1=rcol(97), scalar2=None, op0=ALU.is_equal))
            V(lambda e: e.tensor_tensor(out=rcol(98), in0=rcol(97), in1=rcol(96), op=ALU.subtract))
            P.op("act", lambda e: e.activation(out=rcol(99), in_=rcol(98), func=AF.Exp), r=[k], w=[k])
            V(lambda e: e.tensor_scalar(out=rcol(100), in0=rcol(99), scalar1=1.0, scalar2=None, op0=ALU.add))
            V(lambda e: e.reciprocal(out=rcol(101), in_=rcol(100)))
            V(lambda e: e.tensor_tensor(out=rcol(102), in0=rcol(101), in1=rcol(42), op=ALU.mult))
            V(lambda e: e.tensor_tensor(out=rcol(103), in0=rcol(102), in1=rcol(99), op=ALU.mult))
            V(lambda e: e.tensor_scalar(out=rcol(224, 32), in0=rcol(128, 32), scalar1=rcol(102), scalar2=None, op0=ALU.mult))
            P.op("dve", lambda e: e.scalar_tensor_tensor(out=comb[:, bl, :], in0=rcol(192, 32), scalar=rcol(103), in1=rcol(224, 32), op0=ALU.mult, op1=ALU.add),
                 r=[k], w=["comb"])


        w1v = w1.rearrange("e (c p) n -> e p c n", p=128)
        w3v = w3.rearrange("e (c p) n -> e p c n", p=128)
        w2v = w2.rearrange("e (c p) n -> e p c n", p=128)
        slot13 = [0]
        slot2 = [0]
        loaded13 = {}
        loaded2 = {}

        def load13(e_, hh):
            s_ = slot13[0] % 3
            slot13[0] += 1
            loaded13[(e_, hh)] = s_
            P.dma("pool", w13[s_][:, 0, :, :], w1v[e_, :, :, hh * 256:(hh + 1) * 256], w=[("w13", s_)])
            P.dma("pool", w13[s_][:, 1, :, :], w3v[e_, :, :, hh * 256:(hh + 1) * 256], w=[("w13", s_)])

        def load2(e_, kh):
            s_ = slot2[0] % 3
            slot2[0] += 1
            loaded2[(e_, kh)] = s_
            P.dma("pool", w2b[s_], w2v[e_, :, kh * 2:(kh + 1) * 2, :], w=[("w2", s_)])

        def H_phase(e_):
            hb = hid[e_ % 2]
            hk = ("hid", e_ % 2)
            cnt = 0
            for hh in range(2):
                s_ = loaded13[(e_, hh)]
                for tg in range(2):
                    for mm in range(2):
                        m = 2 * hh + mm
                        b1 = (cnt % 2) * 2
                        b3 = b1 + 1
                        cnt += 1
                        for c in range(16):
                            P.op("pe", lambda e: e.matmul(ps[b1], lhsT=w13[s_][:, 0, c, mm * 128:(mm + 1) * 128], rhs=hnT[:, c, tg * 512:(tg + 1) * 512],
                                                          start=(c == 0), stop=(c == 15)), r=[("w13", s_), "hnT"], w=[("ps", b1)], signal=(c == 15))
                        for c in range(16):
                            P.op("pe", lambda e: e.matmul(ps[b3], lhsT=w13[s_][:, 1, c, mm * 128:(mm + 1) * 128], rhs=hnT[:, c, tg * 512:(tg + 1) * 512],
                                                          start=(c == 0), stop=(c == 15)), r=[("w13", s_), "hnT"], w=[("ps", b3)], signal=(c == 15))
                        sl = sil[cnt % 2]
                        P.op("act", lambda e: e.activation(out=sl, in_=ps[b1], func=AF.Silu), r=[("ps", b1)], w=[("sil", cnt % 2)])
                        P.op("dve", lambda e: e.tensor_tensor(out=hb[:, tg, m, :], in0=sl, in1=ps[b3], op=ALU.mult),
                             r=[("sil", cnt % 2), ("ps", b3)], w=[hk])
                        if e_ == 0:
                            route_block(cnt - 1)

        ycnt = [0]

        def Y_phase(e_):
            hb = hid[e_ % 2]
            hk = ("hid", e_ % 2)
            for bl in range(8):
                tg = bl // 4
                tb_ = bl % 4
                for cg in range(4):
                    by = 4 + ycnt[0] % 3
                    ycnt[0] += 1
                    for m in range(4):
                        s_ = loaded2[(e_, m // 2)]
                        P.op("pe", lambda e: e.matmul(ps[by], lhsT=hb[:, tg, m, tb_ * 128:(tb_ + 1) * 128], rhs=w2b[s_][:, m % 2, cg * 512:(cg + 1) * 512],
                                                      start=(m == 0), stop=(m == 3)), r=[hk, ("w2", s_)], w=[("ps", by)], signal=(m == 3))
                    hsl = h1[:, bl, cg * 512:(cg + 1) * 512]
                    P.op("dve", lambda e: e.scalar_tensor_tensor(out=hsl, in0=ps[by], scalar=comb[:, bl, e_:e_ + 1], in1=hsl, op0=ALU.mult, op1=ALU.add),
                         r=[("ps", by), "comb", ("h1", bl)], w=[("h1", bl)])

        NEX = NE if stage >= 6 else 2
        load13(0, 0)
        load13(0, 1)
        load2(0, 0)
        for e_ in range(NEX):
            if e_ + 1 < NEX:
                load13(e_ + 1, 0)
            load2(e_, 1)
            H_phase(e_)
            if e_ + 1 < NEX:
                load13(e_ + 1, 1)
                load2(e_ + 1, 0)
            Y_phase(e_)

        if stage <= 6:
            for bl in range(8):
                P.dma("sp", out_d[bl * 128:(bl + 1) * 128, :], h1[:, bl, :], r=[("h1", bl)])
            return _finish(nc, P, out_d)
        P.barrier()
        P.release(m_d)
        hi_save = P.mark()
        P.release(m_persist)
        gple = P.alloc("gple_bc", [128, D], F32)
        bple = P.alloc("bple_bc", [128, D], F32)
        wpl = P.alloc("wpl", [128, 2, D], BF16)
        xs0 = P.alloc("xs_f0", [128, D], BF16)
        xs1 = P.alloc("xs_f1", [128, D], BF16)
        assert P.mark() <= m_keep
        P.release(hi_save)
        wg = [P.alloc(f"wg{i}", [128, 16, 512], BF16) for i in range(2)]
        pT = P.alloc("pT", [128, 2, TOK], BF16)
        pblk = [P.alloc(f"pblk{i}", [128, 256], F32) for i in range(2)]
        pbf = [P.alloc(f"pbf{i}", [128, 256], BF16) for i in range(2)]
        pre = [P.alloc(f"pre{i}", [128, 512], F32) for i in range(2)]
        obuf = [P.alloc(f"obuf{i}", [128, 512], F32) for i in range(3)]
        P.dma("sp", gple, prow[0:1, R_NPLE:R_NPLE + D].partition_broadcast(128), w=["gple"])
        P.dma("sp", bple, prow[0:1, R_BPLE:R_BPLE + D].partition_broadcast(128), w=["bple"])
        P.dma("pool", wpl, w_ple.rearrange("(c p) n -> p c n", p=128), w=["wpl"])
        w_pg_v = w_pg.rearrange("(c p) n -> p c n", p=128)
        for bl in range(8):
            rmsnorm_T(h1[:, bl, :], 128, gple, "gple", hnT, bl * 128, ("h1", bl), "hnT", [xs0, xs1][bl % 2], ("xs", bl % 2), banks=[4, 5])
            P.dma("sp", pblk[bl % 2], po[bl * 128:(bl + 1) * 128, :], w=[("pblk", bl % 2)])
            P.op("dve", lambda e: e.tensor_copy(out=pbf[bl % 2], in_=pblk[bl % 2]), r=[("pblk", bl % 2)], w=[("pbf", bl % 2)])
            for c in range(2):
                P.op("pe", lambda e: e.transpose(out=psb[6][:, c * 128:(c + 1) * 128], in_=pbf[bl % 2][:, c * 128:(c + 1) * 128], identity=ident),
                     r=[("pbf", bl % 2), "ident"], w=[("ps", 6)], signal=(c == 1))
            P.op("act", lambda e: e.copy(out=pT[:, :, bl * 128:(bl + 1) * 128], in_=psb[6][:, 0:256].rearrange("p (c t) -> p c t", c=2)),
                 r=[("ps", 6)], w=["pT"])
        oc = 0
        for cg in range(4):
            wgb = wg[cg % 2]
            wgk = ("wg", cg % 2)
            P.dma("pool", wgb[:, 0:8, :], w_pg_v[:, 0:8, cg * 512:(cg + 1) * 512], w=[wgk])
            P.dma("pool", wgb[:, 8:16, :], w_pg_v[:, 8:16, cg * 512:(cg + 1) * 512], w=[wgk])
            for bl in range(8):
                bg = (bl % 2) * 2
                bp = bg + 1
                for c in range(16):
                    P.op("pe", lambda e: e.matmul(ps[bg], lhsT=hnT[:, c, bl * 128:(bl + 1) * 128], rhs=wgb[:, c, :], start=(c == 0), stop=(c == 15)),
                         r=["hnT", wgk], w=[("ps", bg)], signal=(c == 15))
                for c in range(2):
                    P.op("pe", lambda e: e.matmul(ps[bp], lhsT=pT[:, c, bl * 128:(bl + 1) * 128], rhs=wpl[:, c, cg * 512:(cg + 1) * 512], start=(c == 0), stop=(c == 1)),
                         r=["pT", "wpl"], w=[("ps", bp)], signal=(c == 1))
                pr = pre[bl % 2]
                prk = ("pre", bl % 2)
                P.op("dve", lambda e: e.tensor_tensor(out=pr, in0=ps[bg], in1=bple[:, cg * 512:(cg + 1) * 512], op=ALU.add), r=[("ps", bg), "bple"], w=[prk])
                P.op("act", lambda e: e.activation(out=pr, in_=pr, func=AF.Sigmoid), r=[prk], w=[prk])
                P.op("dve", lambda e: e.tensor_tensor(out=pr, in0=pr, in1=ps[bp], op=ALU.mult), r=[prk, ("ps", bp)], w=[prk])
                ob_ = obuf[oc % 3]
                obk = ("obuf", oc % 3)
                oc += 1
                P.op("dve", lambda e: e.tensor_tensor(out=ob_, in0=pr, in1=h1[:, bl, cg * 512:(cg + 1) * 512], op=ALU.add), r=[prk, ("h1", bl)], w=[obk])
                P.dma("sp", out_d[bl * 128:(bl + 1) * 128, cg * 512:(cg + 1) * 512], ob_, r=[obk])
        return _finish(nc, P, out_d)


def _finish(nc, P, out_d):
    P.barrier()
    return nc, P


_CACHE = {}


def _host_inputs(inp):
    x = np.ascontiguousarray(inp["x"], dtype=np.float32)
    p = inp["p"]
    pos = inp["positions"].astype(np.int32)
    f32 = np.float32
    prow = np.concatenate([inp["norm_mix"][0], inp["g_out_attn"][0], inp["norm_moe"][0], inp["norm_ple"][0], inp["b_ple_gate"][0],
                           inp["b_group"][0], inp["b_router"][0]]).astype(f32)[None, :]
    pc = np.zeros((128, NCOL), f32)
    pc[:, C_GKV:C_GKV + 2] = inp["g_kv_lat"][0].reshape(2, 128).T
    pc[:, C_GQL:C_GQL + 4] = inp["g_q_lat"][0].reshape(4, 128).T
    gq = inp["g_q_head"][0]
    gk = inp["g_k_head"][0]
    pc[:, C_GQN] = gq[:128]
    pc[:, C_GKN] = gk[:128]
    sw = (np.arange(64) + 32) % 64
    pc[:64, C_GQR] = gq[128:]
    pc[:64, C_GQRS] = gq[128:][sw]
    pc[:64, C_GKR] = gk[128:]
    pc[:64, C_GKRS] = gk[128:][sw]
    freq = (f32(10000.0) ** (-(np.arange(0, 64, 2, dtype=f32)) / f32(64))).astype(f32)
    pc[:64, C_FREQ] = np.concatenate([freq, freq])
    pc[:32, C_SGN] = -1.0
    pc[32:64, C_SGN] = 1.0
    wc = inp["w_conv"][0]
    for ct in range(8):
        for j in range(3):
            pc[:, C_WCONV + ct * 3 + j] = wc[j, ct * 128:(ct + 1) * 128]
    pc[:, C_GOC:C_GOC + 8] = inp["g_out_conv"][0].reshape(8, 128).T
    pc[:, C_PHASE] = 0.0
    pc[:, C_PHASE + 1] = EPS
    shared = {
        "pcols": pc, "prow": prow,
        "w_in": np.ascontiguousarray(inp["w_in"][0]),
        "w_uq": np.ascontiguousarray(inp["w_uq"][0].reshape(512, H * 192)),
        "w_ukv": np.ascontiguousarray(inp["w_ukv"][0].reshape(256, H * 256)),
        "w_out": np.ascontiguousarray(inp["w_out"][0]),
        "w_r": np.ascontiguousarray(np.concatenate([inp["w_group"][0], inp["w_router"][0]], axis=1)),
        "w1": np.ascontiguousarray(inp["w1"][0]), "w3": np.ascontiguousarray(inp["w3"][0]), "w2": np.ascontiguousarray(inp["w2"][0]),
        "w_ple": np.ascontiguousarray(inp["w_ple"][0]), "w_pg": np.ascontiguousarray(inp["w_ple_gate"][0]),
    }
    in_maps = []
    idxs = []
    for c in range(8):
        b, j = c // 4, c % 4
        blocks = [4 * i + j for i in range(8)]
        tok = np.concatenate([np.arange(bk * 128, (bk + 1) * 128) for bk in blocks])
        halo = np.zeros((HALO, D), f32)
        for i, bk in enumerate(blocks):
            t0 = bk * 128
            if t0 >= 2:
                halo[2 * i:2 * i + 2] = x[b, t0 - 2:t0]
        xo = np.concatenate([x[b, tok], halo], axis=0)
        mask = np.zeros((128, 4, 128), f32)
        kk = np.arange(128)[:, None] // 64
        qq = np.arange(128)[None, :] // 64
        for r in range(4):
            if r < j:
                mask[:, r, :] = 1.0
            elif r == j:
                mask[:, r, :] = (kk <= qq).astype(f32)
        m = dict(shared)
        m.update({"xb": x[b], "xo": np.ascontiguousarray(xo), "posb": pos[b][None, :], "poso": np.ascontiguousarray(pos[b][tok][None, :]),
                  "po": np.ascontiguousarray(p[0, b][tok]).astype(f32), "mask": mask.reshape(128, 512)})
        in_maps.append(m)
        idxs.append((b, tok))
    return in_maps, idxs


def kernel(**inp):
    if "nc" not in _CACHE:
        _CACHE["nc"] = build()[0]
    nc = _CACHE["nc"]
    in_maps, idxs = _host_inputs(inp)
    res = run_bass_kernel_spmd(nc, in_maps, core_ids=list(range(8)))
    out = np.zeros((2, S, D), np.float32)
    for c, (b, tok) in enumerate(idxs):
        out[b, tok] = res.results[c]["out"]
    return out
```
